# Optimizing a Trainium2 kernel written in Bass

```python
import jax
import jax.numpy as jnp
from jax import lax
import numpy as np

D_MODEL = 1024
BATCH = 4
SEQ = 8192
DEPTH = 1

GMLP_WIDTH = D_MODEL // 2
GMLP_GROUPS = 4
GMLP_CH = GMLP_WIDTH // GMLP_GROUPS
GMLP_CHUNK = 128
ATT_HEADS = 8
HEAD_DIM = 64
ATT_WIDTH = ATT_HEADS * HEAD_DIM
ROPE_DIM = HEAD_DIM // 4
ROPE_THETA = 500000.0
MOBA_BLOCK = 256
MOBA_TOPK = 3
Q_BLOCK = 128
N_GROUPS = 4
EXPERTS_PER_GROUP = 8
N_EXPERTS = N_GROUPS * EXPERTS_PER_GROUP
TOP_K_EXPERT = 2
D_EXPERT = D_MODEL // 2
DISPATCH_BLOCK = 256
N_MOD = 6
PROJ_COLS = 2 * GMLP_WIDTH + 3 * ATT_WIDTH + 2 * D_MODEL
EPS = 1e-6

kernel_name = 'hybrid_gmlp_moba_hmoe_block'


def rmsnorm(x, g):
    xf = x.astype(jnp.float32)
    y = xf * lax.rsqrt(jnp.mean(xf * xf, axis=-1, keepdims=True) + EPS)
    return (y * g.astype(jnp.float32)).astype(x.dtype)


def layernorm(x, g, b):
    xf = x.astype(jnp.float32)
    mu = jnp.mean(xf, axis=-1, keepdims=True)
    var = jnp.mean(jnp.square(xf - mu), axis=-1, keepdims=True)
    y = (xf - mu) * lax.rsqrt(var + EPS) * g.astype(jnp.float32) + b.astype(jnp.float32)
    return y.astype(x.dtype)


def partial_rotary(t, pos):
    half = ROPE_DIM // 2
    inv_freq = jnp.power(ROPE_THETA, -jnp.arange(half, dtype=jnp.float32) * 2.0 / ROPE_DIM)
    ang = pos.astype(jnp.float32)[:, None] * inv_freq[None, :]
    cos = jnp.cos(ang)[None, :, None, :]
    sin = jnp.sin(ang)[None, :, None, :]
    t1 = t[..., :half].astype(jnp.float32)
    t2 = t[..., half:ROPE_DIM].astype(jnp.float32)
    rot = jnp.concatenate([t1 * cos - t2 * sin, t2 * cos + t1 * sin], axis=-1).astype(t.dtype)
    return jnp.concatenate([rot, t[..., ROPE_DIM:]], axis=-1)


def spatial_gating_mixer(p, ln_g, ln_b, w_spatial, b_spatial):
    B, S, _ = p.shape
    z = jax.nn.gelu(p)
    u, v = z[..., :GMLP_WIDTH], z[..., GMLP_WIDTH:]
    v = layernorm(v, ln_g, ln_b)
    v = v.reshape(B, S // GMLP_CHUNK, GMLP_CHUNK, GMLP_GROUPS, GMLP_CH)
    causal = jnp.tril(jnp.ones((GMLP_CHUNK, GMLP_CHUNK), dtype=bool))
    w = jnp.where(causal[None], w_spatial, 0)
    sv = jnp.einsum('gts,bnsgc->bntgc', w, v) + b_spatial.T[None, None, :, :, None]
    return u * sv.reshape(B, S, GMLP_WIDTH)


def moba_attention(q, k, v):
    B, S, H, Dh = q.shape
    S_pad = -(-S // MOBA_BLOCK) * MOBA_BLOCK
    if S_pad != S:
        padw = ((0, 0), (0, S_pad - S), (0, 0), (0, 0))
        q, k, v = jnp.pad(q, padw), jnp.pad(k, padw), jnp.pad(v, padw)
    nb = S_pad // MOBA_BLOCK
    nqb = S_pad // Q_BLOCK
    qpb = MOBA_BLOCK // Q_BLOCK
    ksel = min(MOBA_TOPK, nb)
    q = q.transpose(0, 2, 1, 3) * (Dh ** -0.5)
    k = k.transpose(0, 2, 1, 3)
    v = v.transpose(0, 2, 1, 3)
    k_blocks = k.reshape(B, H, nb, MOBA_BLOCK, Dh)
    v_blocks = v.reshape(B, H, nb, MOBA_BLOCK, Dh)
    k_mean = jnp.mean(k_blocks.astype(jnp.float32), axis=3)
    gate = jnp.einsum('bhtd,bhnd->bhtn', q.astype(jnp.float32), k_mean)
    own_blk = jnp.arange(S_pad) // MOBA_BLOCK
    past = jnp.arange(nb)[None, :] < own_blk[:, None]
    gate = jnp.where(past, gate, -jnp.inf)
    _, sel = lax.top_k(gate, ksel)

    q_steps = q.reshape(B, H, nqb, Q_BLOCK, Dh).transpose(0, 2, 1, 3, 4).reshape(B * nqb, H, Q_BLOCK, Dh)
    sel_steps = sel.reshape(B, H, nqb, Q_BLOCK, ksel).transpose(0, 2, 1, 3, 4).reshape(B * nqb, H, Q_BLOCK, ksel)
    b_ids = jnp.repeat(jnp.arange(B), nqb)
    qb_ids = jnp.tile(jnp.arange(nqb), B)

    def step(args):
        b, qi, q_blk, sel_blk = args
        kb = k_blocks[b]
        vb = v_blocks[b]
        k_sel = jax.vmap(lambda kh, ih: kh[ih])(kb, sel_blk)
        v_sel = jax.vmap(lambda vh, ih: vh[ih])(vb, sel_blk)
        j = qi // qpb
        k_own = lax.dynamic_index_in_dim(kb, j, axis=1, keepdims=False)
        v_own = lax.dynamic_index_in_dim(vb, j, axis=1, keepdims=False)
        q_pos = qi * Q_BLOCK + jnp.arange(Q_BLOCK)
        k_pos = j * MOBA_BLOCK + jnp.arange(MOBA_BLOCK)
        valid = jnp.arange(ksel) < j
        s_sel = jnp.einsum('hqd,hqnld->hqnl', q_blk, k_sel).astype(jnp.float32)
        s_sel = jnp.where(valid[None, None, :, None], s_sel, -jnp.inf).reshape(H, Q_BLOCK, ksel * MOBA_BLOCK)
        s_own = jnp.einsum('hqd,hld->hql', q_blk, k_own).astype(jnp.float32)
        s_own = jnp.where(k_pos[None, None, :] <= q_pos[None, :, None], s_own, -jnp.inf)
        prob = jax.nn.softmax(jnp.concatenate([s_sel, s_own], axis=-1), axis=-1).astype(v.dtype)
        p_sel = prob[..., :ksel * MOBA_BLOCK].reshape(H, Q_BLOCK, ksel, MOBA_BLOCK)
        p_own = prob[..., ksel * MOBA_BLOCK:]
        return jnp.einsum('hqnl,hqnld->hqd', p_sel, v_sel) + jnp.einsum('hql,hld->hqd', p_own, v_own)

    out = lax.map(step, (b_ids, qb_ids, q_steps, sel_steps))
    out = out.reshape(B, nqb, H, Q_BLOCK, Dh).transpose(0, 1, 3, 2, 4).reshape(B, S_pad, H * Dh)
    return out[:, :S]


def hierarchical_moe(h, w_rg, b_rg, w_re, b_re, w_gate, w_up, w_down):
    B, S, D = h.shape
    T = B * S
    xt = h.reshape(T, D)
    g_prob = jax.nn.softmax((xt @ w_rg + b_rg).astype(jnp.float32), axis=-1)
    g_p, g_idx = lax.top_k(g_prob, 1)
    e_logits = (jnp.einsum('td,dge->tge', xt, w_re) + b_re).astype(jnp.float32)
    e_logits = jnp.take_along_axis(e_logits, g_idx[:, :, None], axis=1)[:, 0]
    e_prob = jax.nn.softmax(e_logits, axis=-1)
    e_p, e_idx = lax.top_k(e_prob, TOP_K_EXPERT)
    weights = g_p * e_p / jnp.sum(e_p, axis=-1, keepdims=True)
    expert = g_idx * EXPERTS_PER_GROUP + e_idx

    A = T * TOP_K_EXPERT
    flat_e = expert.reshape(A)
    flat_tok = jnp.repeat(jnp.arange(T), TOP_K_EXPERT)
    flat_w = weights.reshape(A)
    order = jnp.argsort(flat_e)
    e_sorted = flat_e[order]
    tok_sorted = flat_tok[order]
    w_sorted = flat_w[order]
    counts = jnp.bincount(flat_e, length=N_EXPERTS)
    padded = (counts + DISPATCH_BLOCK - 1) // DISPATCH_BLOCK * DISPATCH_BLOCK
    start = jnp.cumsum(counts) - counts
    pend = jnp.cumsum(padded)
    pstart = pend - padded
    dest = pstart[e_sorted] + jnp.arange(A) - start[e_sorted]
    n_blocks = -(-A // DISPATCH_BLOCK) + N_EXPERTS
    P = n_blocks * DISPATCH_BLOCK
    x_buf = jnp.zeros((P, D), h.dtype).at[dest].set(xt[tok_sorted])
    block_expert = jnp.minimum(
        jnp.searchsorted(pend, jnp.arange(n_blocks) * DISPATCH_BLOCK, side='right'), N_EXPERTS - 1)

    def expert_block(args):
        xb, e = args
        hid = jax.nn.silu(xb @ w_gate[e]) * (xb @ w_up[e])
        return hid @ w_down[e]

    y_buf = lax.map(expert_block, (x_buf.reshape(n_blocks, DISPATCH_BLOCK, D), block_expert)).reshape(P, D)
    y = y_buf[dest] * w_sorted[:, None].astype(h.dtype)
    out = jnp.zeros((T, D), h.dtype).at[tok_sorted].add(y)
    return out.reshape(B, S, D)


def setup_inputs(seed: int = 0) -> dict:
    key = jax.random.key(seed)
    ks = jax.random.split(key, 22)
    L = DEPTH

    def nrm(k, shape, scale):
        return jax.random.normal(k, shape, jnp.float32) * scale

    return {
        'x': nrm(ks[0], (BATCH, SEQ, D_MODEL), 1.0),
        'c': nrm(ks[1], (BATCH, D_MODEL), 1.0),
        'ada_w': nrm(ks[2], (L, D_MODEL, N_MOD * D_MODEL), 0.5 * D_MODEL ** -0.5),
        'ada_b': nrm(ks[3], (L, N_MOD * D_MODEL), 0.01),
        'norm1_g': 1.0 + nrm(ks[4], (L, D_MODEL), 0.02),
        'norm2_g': 1.0 + nrm(ks[5], (L, D_MODEL), 0.02),
        'w_in': nrm(ks[6], (L, D_MODEL, PROJ_COLS), D_MODEL ** -0.5),
        'gmlp_ln_g': 1.0 + nrm(ks[7], (L, GMLP_WIDTH), 0.02),
        'gmlp_ln_b': nrm(ks[8], (L, GMLP_WIDTH), 0.02),
        'w_spatial': nrm(ks[9], (L, GMLP_GROUPS, GMLP_CHUNK, GMLP_CHUNK), GMLP_CHUNK ** -0.5),
        'b_spatial': 1.0 + nrm(ks[10], (L, GMLP_GROUPS, GMLP_CHUNK), 0.02),
        'w_branch_a': nrm(ks[11], (L, GMLP_WIDTH, D_MODEL), GMLP_WIDTH ** -0.5),
        'w_branch_b': nrm(ks[12], (L, ATT_WIDTH, D_MODEL), ATT_WIDTH ** -0.5),
        'w_out': nrm(ks[13], (L, D_MODEL, D_MODEL), D_MODEL ** -0.5),
        'w_router_group': nrm(ks[14], (L, D_MODEL, N_GROUPS), D_MODEL ** -0.5),
        'b_router_group': nrm(ks[15], (L, N_GROUPS), 0.01),
        'w_router_expert': nrm(ks[16], (L, D_MODEL, N_GROUPS, EXPERTS_PER_GROUP), D_MODEL ** -0.5),
        'b_router_expert': nrm(ks[17], (L, N_GROUPS, EXPERTS_PER_GROUP), 0.01),
        'w_gate': nrm(ks[18], (L, N_EXPERTS, D_MODEL, D_EXPERT), D_MODEL ** -0.5),
        'w_up': nrm(ks[19], (L, N_EXPERTS, D_MODEL, D_EXPERT), D_MODEL ** -0.5),
        'w_down': nrm(ks[20], (L, N_EXPERTS, D_EXPERT, D_MODEL), D_EXPERT ** -0.5),
        'final_norm_g': 1.0 + nrm(ks[21], (D_MODEL,), 0.02),
    }


def reference(x, c, ada_w, ada_b, norm1_g, norm2_g, w_in, gmlp_ln_g, gmlp_ln_b, w_spatial, b_spatial,
              w_branch_a, w_branch_b, w_out, w_router_group, b_router_group, w_router_expert,
              b_router_expert, w_gate, w_up, w_down, final_norm_g):
    B, S, _ = x.shape
    pos = jnp.arange(S)
    splits = [2 * GMLP_WIDTH, 2 * GMLP_WIDTH + ATT_WIDTH, 2 * GMLP_WIDTH + 2 * ATT_WIDTH,
              2 * GMLP_WIDTH + 3 * ATT_WIDTH, 2 * GMLP_WIDTH + 3 * ATT_WIDTH + D_MODEL]
    for l in range(DEPTH):
        mod = c @ ada_w[l] + ada_b[l]
        sh1, sc1, g1, sh2, sc2, g2 = jnp.split(mod, N_MOD, axis=-1)
        h = rmsnorm(x, norm1_g[l]) * (1.0 + sc1[:, None]) + sh1[:, None]
        proj = h @ w_in[l]
        p_a, q, k, v, gate_a, gate_b = jnp.split(proj, splits, axis=-1)
        y_a = spatial_gating_mixer(p_a, gmlp_ln_g[l], gmlp_ln_b[l], w_spatial[l], b_spatial[l]) @ w_branch_a[l]
        q = partial_rotary(q.reshape(B, S, ATT_HEADS, HEAD_DIM), pos)
        k = partial_rotary(k.reshape(B, S, ATT_HEADS, HEAD_DIM), pos)
        v = v.reshape(B, S, ATT_HEADS, HEAD_DIM)
        y_b = moba_attention(q, k, v) @ w_branch_b[l]
        merged = jax.nn.sigmoid(gate_a) * y_a + jax.nn.sigmoid(gate_b) * y_b
        x = x + g1[:, None] * (merged @ w_out[l])
        h = rmsnorm(x, norm2_g[l]) * (1.0 + sc2[:, None]) + sh2[:, None]
        x = x + g2[:, None] * hierarchical_moe(h, w_router_group[l], b_router_group[l], w_router_expert[l],
                                               b_router_expert[l], w_gate[l], w_up[l], w_down[l])
    return rmsnorm(x, final_norm_g)
```

```python
import numpy as np
from contextlib import ExitStack
import concourse.bass as bass
import concourse.mybir as mybir
from concourse.bass_utils import run_bass_kernel_spmd

F32 = mybir.dt.float32
BF16 = mybir.dt.bfloat16
I32 = mybir.dt.int32
AF = mybir.ActivationFunctionType
ALU = mybir.AluOpType
AX = mybir.AxisListType

NPAIR = 16
NOT = 32
NSLOT = 64
NEG = -30000.0


class Buf:
    __slots__ = ("last_w", "readers")

    def __init__(self):
        self.last_w = None
        self.readers = []


class PBuf(Buf):
    __slots__ = ("acc",)

    def __init__(self):
        super().__init__()
        self.acc = {}


class Eng:
    def __init__(self, name, h, sem, in_order=False):
        self.name, self.h, self.sem, self.in_order = name, h, sem, in_order
        self.cnt = 0
        self.waited = {}
        self.pending = False
        self.n_instr = 0


class FW:
    def __init__(self, nc, stack, n_dma_sems=48):
        self.nc = nc
        self.engs = {}
        for name, h, io in (("pe", nc.tensor, True), ("act", nc.scalar, False),
                            ("dve", nc.vector, False), ("pool", nc.gpsimd, False),
                            ("sp", nc.sync, False)):
            sem = stack.enter_context(nc.semaphore("s_" + name))
            self.engs[name] = Eng(name, h, sem, io)
        self.rings = {"sp": [], "pool": [], "act": []}
        for q, n in (("sp", 32), ("pool", 24), ("act", 12)):
            for i in range(n):
                sem = stack.enter_context(nc.semaphore("d%s%d" % (q, i)))
                self.rings[q].append([sem, 0])
        self.dma_ring = self.rings["sp"] + self.rings["pool"] + self.rings["act"]
        self.dma_i = {"sp": 0, "pool": 0, "act": 0}

    def _deps(self, eng, reads, writes):
        deps = {}

        def add(ev):
            if ev is None:
                return
            sem, val = ev
            if sem is eng.sem and eng.in_order:
                return
            k = id(sem)
            if k not in deps or deps[k][1] < val:
                deps[k] = (sem, val)
        for b in reads:
            add(b.last_w)
            if isinstance(b, PBuf):
                for nm, ev in b.acc.items():
                    if nm != eng.name:
                        add(ev)
        for b in writes:
            add(b.last_w)
            for r in b.readers:
                add(r)
            if isinstance(b, PBuf):
                for nm, ev in b.acc.items():
                    if nm != eng.name:
                        add(ev)
        return deps

    def _wait(self, eng, deps):
        for k, (sem, val) in deps.items():
            if eng.waited.get(k, 0) < val:
                eng.h.wait_ge(sem, val)
                eng.waited[k] = val
                eng.n_instr += 1

    def _update(self, ev, reads, writes, engname=None):
        for b in writes:
            b.last_w = ev
            b.readers = []
            if isinstance(b, PBuf):
                b.acc[engname] = ev
        for b in reads:
            if b in writes:
                continue
            if isinstance(b, PBuf):
                b.acc[engname] = ev
                continue
            b.readers.append(ev)
            if len(b.readers) > 48:
                best = {}
                for (s, v) in b.readers:
                    k = id(s)
                    if k not in best or best[k][1] < v:
                        best[k] = (s, v)
                b.readers = list(best.values())

    def op(self, engname, fn, reads=(), writes=(), signal=True):
        eng = self.engs[engname]
        self._wait(eng, self._deps(eng, reads, writes))
        ins = fn(eng.h)
        eng.n_instr += 1
        if signal:
            ins.then_inc(eng.sem, 1)
            eng.cnt += 1
            ev = (eng.sem, eng.cnt)
            eng.pending = False
        else:
            ev = (eng.sem, eng.cnt + 1)
            eng.pending = True
        self._update(ev, reads, writes, engname)

    def dma(self, qname, out, in_, reads=(), writes=(), indirect=None):
        eng = self.engs[qname]
        deps = self._deps(eng, reads, writes)
        ring = self.rings[qname]
        slot = ring[self.dma_i[qname] % len(ring)]
        self.dma_i[qname] += 1
        if slot[1] > 0:
            k = id(slot[0])
            if k not in deps or deps[k][1] < slot[1]:
                deps[k] = (slot[0], slot[1])
        self._wait(eng, deps)
        if indirect is None:
            ins = eng.h.dma_start(out=out, in_=in_)
        else:
            ins = eng.h.indirect_dma_start(out=out, in_=in_, **indirect)
        eng.n_instr += 1
        slot[1] += 16
        ins.then_inc(slot[0], 16)
        self._update((slot[0], slot[1]), reads, writes)

    def barrier(self):
        assert not self.engs["pe"].pending
        for e in self.engs.values():
            deps = {}
            for f in self.engs.values():
                if f is not e and f.cnt > 0:
                    deps[id(f.sem)] = (f.sem, f.cnt)
            for s in self.dma_ring:
                if s[1] > 0:
                    deps[id(s[0])] = (s[0], s[1])
            self._wait(e, deps)


class _Stop(Exception):
    pass


def build(debug=False, upto=99):
    nc = bass.Bass("TRN2", target_bir_lowering=False)

    def din(name, shape, dt=F32):
        return nc.dram_tensor(name, list(shape), dt, kind="ExternalInput").ap()

    skind = "ExternalOutput" if debug else "Internal"

    def dscr(name, shape, dt):
        return nc.dram_tensor(name, list(shape), dt, kind=skind).ap()

    x_own = din("x_own", [4096, 1024])
    x_oth = din("x_oth", [4096, 1024])
    rot_own = din("rot_own", [4096, 128])
    rot_oth = din("rot_oth", [4096, 128])
    pastb = din("pastb", [1, 512])
    c_pk = din("c_pk", [128, 8])
    ada_w = din("ada_w", [1024, 6144])
    ada_b = din("ada_b", [1, 6144])
    norm1_g = din("norm1_g", [1, 1024])
    norm2_g = din("norm2_g", [1, 1024])
    final_g = din("final_g", [1, 1024])
    w_in = din("w_in", [1024, 4608])
    ln_g = din("ln_g", [1, 512])
    ln_b = din("ln_b", [1, 512])
    w_sp = din("w_sp", [4, 128, 128])
    b_spT = din("b_spT", [128, 4])
    w_ba = din("w_ba", [512, 1024])
    w_bb = din("w_bb", [512, 1024])
    w_out = din("w_out", [1024, 1024])
    w_rt = din("w_rt", [1024, 36])
    b_rt = din("b_rt", [1, 36])
    w_gate = din("w_gate", [32, 1024, 512])
    w_up = din("w_up", [32, 1024, 512])
    w_down = din("w_down", [32, 512, 1024])
    c_ident = din("c_ident", [128, 128])
    c_tri = din("c_tri", [128, 128])
    c_tris = din("c_tris", [128, 128])
    c_onehot = din("c_onehot", [32, 8192])
    c_misc = din("c_misc", [128, 96])
    out = nc.dram_tensor("out", [4096, 1024], F32, kind="ExternalOutput").ap()

    KT_d = dscr("KT_d", [4, 128, 8192], BF16)
    V_d = dscr("V_d", [8192, 512], BF16)
    QT_d = dscr("QT_d", [8, 96, 4096], BF16)
    GA_d = dscr("GA_d", [4096, 1024], F32)
    GB_d = dscr("GB_d", [4096, 1024], F32)
    X1_d = dscr("X1_d", [4096, 1024], F32)
    H2_d = dscr("H2_d", [4096, 1024], BF16)
    XS_d = dscr("XS_d", [NSLOT * 256, 1024], BF16)
    Y_d = dscr("Y_d", [NSLOT * 256, 1024], F32)
    WB_d = [nc.dram_tensor("WB%d_d" % i, [8192, 4096], BF16, kind="Internal").ap() for i in range(3)]
    AT_d = dscr("AT_d", [4096, 512], BF16) if debug else None
    RT_d = dscr("RT_d", [4096, 8], F32) if debug else None

    with ExitStack() as gs:
      try:
        fw = FW(nc, gs)
        gs_fw = [fw]

        def SB(st, name, shape, dt):
            return st.enter_context(nc.sbuf_tensor(name, list(shape), dt))

        def PS(st, name, shape, dt):
            return st.enter_context(nc.psum_tensor(name, list(shape), dt))

        def MM(o, lhsT, rhs, start=True, stop=True, r=(), w=(), sig=True):
            fw.op("pe", lambda e: e.matmul(o, lhsT=lhsT, rhs=rhs, start=start, stop=stop), r, w, sig)

        def TR(o, i, ident, r=(), w=(), sig=True):
            fw.op("pe", lambda e: e.transpose(out=o, in_=i, identity=ident), r, w, sig)

        def ACT(o, i, func, r=(), w=(), **kw):
            fw.op("act", lambda e: e.activation(out=o, in_=i, func=func, **kw), r, w)

        def CP(eng, o, i, r=(), w=()):
            if eng == "act":
                fw.op("act", lambda e: e.copy(out=o, in_=i), r, w)
            else:
                fw.op(eng, lambda e: e.tensor_copy(out=o, in_=i), r, w)

        def TT(eng, o, a, b, op, r=(), w=()):
            fw.op(eng, lambda e: e.tensor_tensor(out=o, in0=a, in1=b, op=op), r, w)

        def TS(eng, o, a, s1, s2, op0, op1=None, r=(), w=(), **kw):
            if op1 is None:
                fw.op(eng, lambda e: e.tensor_scalar(out=o, in0=a, scalar1=s1, scalar2=None, op0=op0, **kw), r, w)
            else:
                fw.op(eng, lambda e: e.tensor_scalar(out=o, in0=a, scalar1=s1, scalar2=s2, op0=op0, op1=op1, **kw), r, w)

        def STT(o, a, s, b, op0, op1, r=(), w=()):
            fw.op("dve", lambda e: e.scalar_tensor_tensor(out=o, in0=a, scalar=s, in1=b, op0=op0, op1=op1), r, w)

        def RSTD(src, bsrc, ss_, bss, div):
            fw.op("dve", lambda e: e.scalar_tensor_tensor(out=junk_g[:], in0=src, scalar=1.0, in1=src, op0=ALU.mult,
                                                          op1=ALU.mult, accum_out=ss_[:, 0:1]), [bsrc], [b_junk_g, bss])
            TS("dve", ss_[:, 1:2], ss_[:, 0:1], 1.0 / div, 1e-6, ALU.mult, ALU.add, r=[bss], w=[bss])
            TT("pool", ss_[:, 2:3], ss_[:, 1:2], misc[:, 81:82], ALU.pow, r=[bss, b_misc], w=[bss])

        def MEMSET(eng, o, v, w=()):
            fw.op(eng, lambda e: e.memset(o, v), (), w)

        def RED(o, i, op, r=(), w=()):
            fw.op("dve", lambda e: e.tensor_reduce(out=o, in_=i, axis=AX.X, op=op), r, w)

        def chk(k):
            if upto < k:
                fw.barrier()
                raise _Stop()

        ident_bf = SB(gs, "ident_bf", [128, 128], BF16); b_identbf = Buf()
        ident_f = SB(gs, "ident_f", [128, 128], F32); b_identf = Buf()
        tri_bf = SB(gs, "tri_bf", [128, 128], BF16); b_tri = Buf()
        tris_bf = SB(gs, "tris_bf", [128, 128], BF16); b_tris = Buf()
        ones_bf = SB(gs, "ones_bf", [128, 128], BF16); b_onesbf = Buf()
        ones_f = SB(gs, "ones_f", [128, 128], F32); b_onesf = Buf()
        misc = SB(gs, "misc", [128, 96], F32); b_misc = Buf()
        mod = SB(gs, "mod", [128, 6144], F32); b_mod = Buf()
        gfin = SB(gs, "gfin", [128, 1024], F32); b_gfin = Buf()
        junk_g = SB(gs, "junk_g", [128, 1024], BF16); b_junk_g = Buf()
        fw.dma("pool", ident_bf[:], c_ident, writes=[b_identbf])
        fw.dma("sp", ident_f[:], c_ident, writes=[b_identf])
        fw.dma("pool", tri_bf[:], c_tri, writes=[b_tri])
        fw.dma("pool", tris_bf[:], c_tris, writes=[b_tris])
        fw.dma("sp", misc[:], c_misc, writes=[b_misc])
        fw.dma("sp", gfin[:], final_g.partition_broadcast(128), writes=[b_gfin])
        MEMSET("dve", ones_bf[:], 1.0, w=[b_onesbf])
        MEMSET("dve", ones_f[:], 1.0, w=[b_onesf])
        SH1, G1, GT1, SH2, G2, GT2 = [mod[:, i * 1024:(i + 1) * 1024] for i in range(6)]

        chk(0)
        wst = ExitStack()
        win = SB(wst, "win", [128, 8, 4608], BF16); b_win = [Buf() for _ in range(8)]
        wba = SB(wst, "wba", [128, 4, 1024], BF16); b_wba = Buf()
        with ExitStack() as ph:
            adaw = [SB(ph, "adaw%d" % i, [128, 8, 1536], BF16) for i in range(2)]; b_adaw = [Buf(), Buf()]
            adab = SB(ph, "adab", [1, 6144], BF16); b_adab = Buf()
            c_sb = SB(ph, "c_sb", [128, 8], F32); b_csb = Buf()
            cT = SB(ph, "cT", [128, 8, 128], BF16); b_cT = Buf()
            ng = SB(ph, "ng", [128, 2048], F32); b_ng = Buf()
            pm = [PS(ph, "pm%d" % i, [128, 512], F32) for i in range(2)]; b_pm = [PBuf(), PBuf()]
            fw.dma("sp", c_sb[:], c_pk, writes=[b_csb])
            adv = ada_w.rearrange("(kc p) f -> p kc f", p=128)

            def load_ada(q):
                fw.dma("pool", adaw[q % 2][:], adv[:, :, q * 1536:(q + 1) * 1536], writes=[b_adaw[q % 2]])
            load_ada(0)
            load_ada(1)
            fw.dma("pool", adab[:], ada_b, writes=[b_adab])
            fw.dma("sp", ng[:, 0:1024], norm1_g.partition_broadcast(128), writes=[b_ng])
            fw.dma("sp", ng[:, 1024:2048], norm2_g.partition_broadcast(128), writes=[b_ng])
            for kc in range(8):
                CP("dve", cT[:, kc, :], c_sb[:, kc:kc + 1].to_broadcast([128, 128]), r=[b_csb], w=[b_cT])
            for n in range(12):
                q = n // 3
                p = pm[n % 2]; bp = b_pm[n % 2]
                c0 = (n % 3) * 512
                for kc in range(8):
                    MM(p[:, :], cT[:, kc, :], adaw[q % 2][:, kc, c0:c0 + 512], start=(kc == 0), stop=False,
                       r=[b_cT, b_adaw[q % 2]], w=[bp], sig=False)
                MM(p[:, :], ones_bf[0:1, :], adab[0:1, n * 512:(n + 1) * 512], start=False, stop=True,
                   r=[b_onesbf, b_adab], w=[bp])
                CP("act", mod[:, n * 512:(n + 1) * 512], p[:, :], r=[bp], w=[b_mod])
                if n % 3 == 2 and q + 2 < 4:
                    load_ada(q + 2)
                if n == 2:
                    for kc in range(8):
                        fw.dma("pool", win[:, kc, :], w_in[kc * 128:(kc + 1) * 128, :], writes=[b_win[kc]])
                    fw.dma("pool", wba[:], w_ba.rearrange("(kc p) f -> p kc f", p=128), writes=[b_wba])
            STT(G1, G1, 1.0, ng[:, 0:1024], ALU.add, ALU.mult, r=[b_mod, b_ng], w=[b_mod])
            STT(G2, G2, 1.0, ng[:, 1024:2048], ALU.add, ALU.mult, r=[b_mod, b_ng], w=[b_mod])
            fw.barrier()

        chk(1)
        with ExitStack() as ph:
            def D2(name, shape, dt):
                return [SB(ph, "%s%d" % (name, i), shape, dt) for i in range(2)], [Buf(), Buf()]
            wsp_f = SB(ph, "wsp_f", [128, 4, 128], F32); b_wspf = Buf()
            wspT = SB(ph, "wspT", [128, 4, 128], BF16); b_wspT = Buf()
            bsp = SB(ph, "bsp", [128, 4], F32); b_bsp = Buf()
            lng = SB(ph, "lng", [128, 512], F32); b_lng = Buf()
            lnb = SB(ph, "lnb", [128, 512], F32); b_lnb = Buf()
            pastb_sb = SB(ph, "pastb_sb", [128, 16, 32], F32); b_pastb = Buf()
            kmbd = SB(ph, "kmbd", [128, 4, 64], F32); b_kmbd = Buf()
            ksum0 = SB(ph, "ksum0", [128, 4], F32); b_ksum0 = Buf()
            xt, b_xt = D2("xt", [128, 1024], F32)
            rot, b_rot = D2("rot", [128, 128], F32)
            junk = SB(ph, "junk", [128, 1024], BF16); b_junk = Buf()
            ss = SB(ph, "ss", [128, 8], F32); b_ss = Buf()
            t32 = SB(ph, "t32", [128, 1024], F32); b_t32 = Buf()
            hb, b_hb = D2("hb", [128, 1024], BF16)
            hT, b_hT = D2("hT", [128, 8, 128], BF16)
            kr32, b_kr32 = D2("kr32", [128, 8, 64], F32)
            krb, b_krb = D2("krb", [128, 512], BF16)
            qr32, b_qr32 = D2("qr32", [128, 8, 64], F32)
            rtmp = [SB(ph, "rtmp%d" % i, [128, 8, 8], F32) for i in range(4)]; b_rtmp = [Buf() for _ in range(4)]
            vb = SB(ph, "vb", [128, 512], BF16); b_vb = Buf()
            kTs = SB(ph, "kTs", [128, 4, 128], BF16); b_kTs = Buf()
            qT32 = SB(ph, "qT32", [128, 4, 128], F32); b_qT32 = Buf()
            QA, b_QA = D2("QA", [128, 8, 96], BF16)
            gm = SB(ph, "gm", [128, 8, 32], F32); b_gm = Buf()
            top8 = SB(ph, "top8", [128, 8, 8], F32); b_top8 = Buf()
            thr = SB(ph, "thr", [128, 8], F32); b_thr = Buf()
            selm = SB(ph, "selm", [128, 8, 32], F32); b_selm = Buf()
            qtas = SB(ph, "qtas", [96, 8, 128], BF16); b_qtas = Buf()
            ug, b_ug = D2("ug", [128, 512], F32)
            vg = SB(ph, "vg", [128, 512], F32); b_vg = Buf()
            st6 = SB(ph, "st6", [128, 6], F32); b_st6 = Buf()
            mv = SB(ph, "mv", [128, 4], F32); b_mv = Buf()
            vn1 = SB(ph, "vn1", [128, 512], F32); b_vn1 = Buf()
            vnb, b_vnb = D2("vnb", [128, 512], BF16)
            zzb = SB(ph, "zzb", [128, 512], BF16); b_zzb = Buf()
            zTs = SB(ph, "zTs", [128, 4, 128], BF16); b_zTs = Buf()
            sga, b_sga = D2("sga", [128, 1024], F32)
            sgb = SB(ph, "sgb", [128, 1024], F32); b_sgb = Buf()
            GAt = SB(ph, "GAt", [128, 1024], F32); b_GAt = Buf()
            pT = PS(ph, "pT", [128, 1024], BF16); b_pT = PBuf()
            NPB = 3
            pP = [PS(ph, "pP%d" % i, [128, 512], F32) for i in range(NPB)]; b_pP = [PBuf() for _ in range(NPB)]
            pM = PS(ph, "pM", [128, 512], F32); b_pM = PBuf()
            pT2 = PS(ph, "pT2", [128, 1024], BF16); b_pT2 = PBuf()
            pQ = PS(ph, "pQ", [128, 512], F32); b_pQ = PBuf()
            pQA = PS(ph, "pQA", [128, 1024], BF16); b_pQA = PBuf()
            pSV = pQ; b_pSV = b_pQ

            fw.dma("sp", wsp_f[:], w_sp.rearrange("g t s -> t g s"), writes=[b_wspf])
            fw.dma("sp", bsp[:], b_spT, writes=[b_bsp])
            fw.dma("sp", lng[:], ln_g.partition_broadcast(128), writes=[b_lng])
            fw.dma("sp", lnb[:], ln_b.partition_broadcast(128), writes=[b_lnb])
            fw.dma("sp", pastb_sb[:].rearrange("p a b -> p (a b)"), pastb.partition_broadcast(128), writes=[b_pastb])
            MEMSET("dve", kmbd[:], 0.0, w=[b_kmbd])
            for g in range(4):
                TR(pQ[:, g * 128:(g + 1) * 128], wsp_f[:, g, :], ident_f[:], r=[b_wspf, b_identf], w=[b_pQ], sig=(g == 3))
            TT("dve", wspT[:], pQ[:, :].rearrange("p (g t) -> p g t", g=4),
               tri_bf[:].unsqueeze(1).to_broadcast([128, 4, 128]), ALU.mult, r=[b_pQ, b_tri], w=[b_wspT])

            pcnt = [0]

            hpar = [0]

            def proj(c0):
                i = pcnt[0] % NPB
                pcnt[0] += 1
                hj = hpar[0]
                for kc in range(8):
                    MM(pP[i][:, :], hT[hj][:, kc, :], win[:, kc, c0:c0 + 512], start=(kc == 0), stop=(kc == 7),
                       r=[b_hT[hj], b_win[kc]], w=[b_pP[i]], sig=(kc == 7))
                return pP[i], b_pP[i]

            def HTR(n):
                j = n % 2
                for kc in range(8):
                    TR(pT[:, kc * 128:(kc + 1) * 128], hb[j][:, kc * 128:(kc + 1) * 128], ident_bf[:],
                       r=[b_hb[j], b_identbf], w=[b_pT], sig=(kc == 7))
                CP("act", hT[j][:].rearrange("p a b -> p (a b)"), pT[:, :], r=[b_pT], w=[b_hT[j]])

            def rotary(P, bP, dst, b_dst, rt, b_rt):
                Pv = P[:, :].rearrange("p (h d) -> p h d", h=8)
                cos = rt[:, 0:64].rearrange("p (h d) -> p h d", h=8)
                sin = rt[:, 64:128].rearrange("p (h d) -> p h d", h=8)
                CP("act", dst[:], Pv, r=[bP], w=[b_dst])
                TT("dve", rtmp[0][:], Pv[:, :, 0:8], cos, ALU.mult, r=[bP, b_rt], w=[b_rtmp[0]])
                TT("dve", rtmp[1][:], Pv[:, :, 8:16], sin, ALU.mult, r=[bP, b_rt], w=[b_rtmp[1]])
                TT("dve", rtmp[2][:], Pv[:, :, 8:16], cos, ALU.mult, r=[bP, b_rt], w=[b_rtmp[2]])
                TT("dve", rtmp[3][:], Pv[:, :, 0:8], sin, ALU.mult, r=[bP, b_rt], w=[b_rtmp[3]])
                TT("dve", dst[:, :, 0:8], rtmp[0][:], rtmp[1][:], ALU.subtract, r=[b_rtmp[0], b_rtmp[1]], w=[b_dst])
                TT("dve", dst[:, :, 8:16], rtmp[2][:], rtmp[3][:], ALU.add, r=[b_rtmp[2], b_rtmp[3]], w=[b_dst])

            tiles = []
            for i in range(NPAIR):
                for own in (False, True):
                    for tt in range(2):
                        tiles.append((i, own, tt))
            NT = len(tiles)

            def NORM(n):
                i, own, tt = tiles[n]
                j = n % 2
                row = (2 * i + tt) * 128
                xs_, rs_ = (x_own, rot_own) if own else (x_oth, rot_oth)
                fw.dma("pool", xt[j][:], xs_[row:row + 128, :], writes=[b_xt[j]])
                fw.dma("pool", rot[j][:], rs_[row:row + 128, :], writes=[b_rot[j]])
                RSTD(xt[j][:], b_xt[j], ss, b_ss, 1024)
                STT(t32[:], xt[j][:], ss[:, 2:3], G1, ALU.mult, ALU.mult, r=[b_xt[j], b_ss, b_mod], w=[b_t32])
                TT("pool", hb[j][:], t32[:], SH1, ALU.add, r=[b_t32, b_mod], w=[b_hb[j]])

            def HEAD(n, fills=()):
                fills = list(fills)

                def fill():
                    if fills:
                        fills.pop(0)()

                def fill_rest():
                    while fills:
                        fills.pop(0)()
                i, own, tt = tiles[n]
                j = n % 2
                kti = 4 * i + (2 if own else 0) + tt
                ot = 2 * i + tt
                if n == 0:
                    HTR(0)
                hpar[0] = j
                P, bP = proj(1536)
                rotary(P, bP, kr32[j], b_kr32[j], rot[j], b_rot[j])
                CP("pool", krb[j][:], kr32[j][:].rearrange("p h d -> p (h d)"), r=[b_kr32[j]], w=[b_krb[j]])
                fill()
                P, bP = proj(2048)
                CP("act", vb[:], P[:, :], r=[bP], w=[b_vb])
                fw.dma("sp", V_d[kti * 128:(kti + 1) * 128, :], vb[:], reads=[b_vb])
                fill()
                if not own:
                    if n + 1 < NT:
                        HTR(n + 1)
                    fill_rest()
                    return
                P, bP = proj(1024)
                rotary(P, bP, qr32[j], b_qr32[j], rot[j], b_rot[j])
                ACT(QA[j][:, :, 0:64], qr32[j][:], AF.Copy, r=[b_qr32[j]], w=[b_QA[j]], scale=0.125)
                fill()
                P, bP = proj(0)
                ACT(ug[j][:], P[:, :], AF.Gelu_apprx_tanh, r=[bP], w=[b_ug[j]])
                fill()
                P, bP = proj(512)
                ACT(vg[:], P[:, :], AF.Gelu_apprx_tanh, r=[bP], w=[b_vg])
                fill()
                for g in range(2):
                    P, bP = proj(2560 + g * 512)
                    ACT(sga[j][:, g * 512:(g + 1) * 512], P[:, :], AF.Sigmoid, r=[bP], w=[b_sga[j]])
                    if g == 0:
                        fw.op("dve", lambda e: e.bn_stats(out=st6[:], in_=vg[:]), [b_vg], [b_st6])
                        fw.op("dve", lambda e: e.bn_aggr(out=mv[:, 0:2], in_=st6[:]), [b_st6], [b_mv])
                        TS("dve", mv[:, 2:3], mv[:, 1:2], 1e-6, None, ALU.add, r=[b_mv], w=[b_mv])
                        TT("pool", mv[:, 3:4], mv[:, 2:3], misc[:, 81:82], ALU.pow, r=[b_mv, b_misc], w=[b_mv])
                        TS("dve", vn1[:], vg[:], mv[:, 0:1], mv[:, 3:4], ALU.subtract, ALU.mult, r=[b_vg, b_mv], w=[b_vn1])
                        TT("pool", vn1[:], vn1[:], lng[:], ALU.mult, r=[b_vn1, b_lng], w=[b_vn1])
                        TT("dve", vnb[j][:], vn1[:], lnb[:], ALU.add, r=[b_vn1, b_lnb], w=[b_vnb[j]])
                if n + 1 < NT:
                    HTR(n + 1)
                for g in range(2):
                    P, bP = proj(3584 + g * 512)
                    ACT(sgb[:, g * 512:(g + 1) * 512], P[:, :], AF.Sigmoid, r=[bP], w=[b_sgb])
                fw.dma("sp", GB_d[ot * 128:(ot + 1) * 128, :], sgb[:], reads=[b_sgb])
                fill_rest()

            def TAIL(n):
                i, own, tt = tiles[n]
                j = n % 2
                kti = 4 * i + (2 if own else 0) + tt
                pb = 2 * i + (1 if own else 0)
                ot = 2 * i + tt
                def p1():
                    for hp in range(4):
                        TR(pT2[:, hp * 128:(hp + 1) * 128], krb[j][:, hp * 128:(hp + 1) * 128], ident_bf[:],
                           r=[b_krb[j], b_identbf], w=[b_pT2], sig=(hp == 3))
                    CP("act", kTs[:].rearrange("p a b -> p (a b)"), pT2[:, 0:512], r=[b_pT2], w=[b_kTs])
                    fw.dma("sp", KT_d[:, :, kti * 128:(kti + 1) * 128].rearrange("hp r t -> r hp t"), kTs[:], reads=[b_kTs])
                    kr32f = kr32[j][:].rearrange("p h d -> p (h d)")
                    for hp in range(4):
                        MM(pM[:, hp:hp + 1], kr32f[:, hp * 128:(hp + 1) * 128], ones_f[:, 0:1],
                           r=[b_kr32[j], b_onesf], w=[b_pM], sig=(hp == 3))
                    if tt == 0:
                        ACT(ksum0[:], pM[:, 0:4], AF.Copy, r=[b_pM], w=[b_ksum0], scale=1.0 / 256)
                    else:
                        STT(kmbd[0:64, :, pb], pM[0:64, 0:4], 1.0 / 256, ksum0[0:64, :], ALU.mult, ALU.add,
                            r=[b_pM, b_ksum0], w=[b_kmbd])
                        STT(kmbd[64:128, :, 32 + pb], pM[64:128, 0:4], 1.0 / 256, ksum0[64:128, :], ALU.mult, ALU.add,
                            r=[b_pM, b_ksum0], w=[b_kmbd])
                    if not own:
                        return
                    qr32f = qr32[j][:].rearrange("p h d -> p (h d)")
                    for hp in range(4):
                        TR(pQ[:, hp * 128:(hp + 1) * 128], qr32f[:, hp * 128:(hp + 1) * 128], ident_f[:],
                           r=[b_qr32[j], b_identf], w=[b_pQ], sig=(hp == 3))
                    CP("act", qT32[:].rearrange("p a b -> p (a b)"), pQ[:, :], r=[b_pQ], w=[b_qT32])
                    for g in range(4):
                        MM(pSV[:, g * 128:(g + 1) * 128], wspT[:, g, :], vnb[j][:, g * 128:(g + 1) * 128],
                           r=[b_wspT, b_vnb[j]], w=[b_pSV], sig=(g == 3))
                def p2():
                    for hp in range(4):
                        MM(pM[:, 256 + hp * 64:256 + (hp + 1) * 64], qT32[:, hp, :], kmbd[:, hp, :],
                           r=[b_qT32, b_kmbd], w=[b_pM], sig=(hp == 3))
                    for g in range(4):
                        STT(zzb[:, g * 128:(g + 1) * 128], pSV[:, g * 128:(g + 1) * 128], bsp[:, g:g + 1],
                            ug[j][:, g * 128:(g + 1) * 128], ALU.add, ALU.mult, r=[b_pSV, b_bsp, b_ug[j]], w=[b_zzb])
                def p3():
                    TT("dve", gm[:], pM[:, 256:512].rearrange("p (h j) -> p h j", h=8),
                       pastb_sb[:, i, :].unsqueeze(1).to_broadcast([128, 8, 32]), ALU.add, r=[b_pM, b_pastb], w=[b_gm])
                    for h in range(8):
                        fw.op("dve", lambda e, h=h: e.max(out=top8[:, h, :], in_=gm[:, h, :]), [b_gm], [b_top8])
                    TS("dve", thr[:], top8[:, :, 2], -1e29, None, ALU.max, r=[b_top8], w=[b_thr])
                    TT("dve", selm[:], gm[:], thr[:].unsqueeze(2).to_broadcast([128, 8, 32]), ALU.is_ge, r=[b_gm, b_thr], w=[b_selm])
                    TS("dve", QA[j][:, :, 64:96], selm[:], -1.0, -NEG, ALU.add, ALU.mult, r=[b_selm], w=[b_QA[j]])
                    MEMSET("dve", QA[j][:, :, 64 + pb:65 + pb], 0.0, w=[b_QA[j]])
                    for kc in range(4):
                        TR(pT2[:, 512 + kc * 128:512 + (kc + 1) * 128], zzb[:, kc * 128:(kc + 1) * 128], ident_bf[:],
                           r=[b_zzb, b_identbf], w=[b_pT2], sig=(kc == 3))
                    CP("act", zTs[:].rearrange("p a b -> p (a b)"), pT2[:, 512:1024], r=[b_pT2], w=[b_zTs])
                def p4():
                    for g in range(2):
                        jj = pcnt[0] % NPB
                        pcnt[0] += 1
                        for kc in range(4):
                            MM(pP[jj][:, :], zTs[:, kc, :], wba[:, kc, g * 512:(g + 1) * 512], start=(kc == 0), stop=(kc == 3),
                               r=[b_zTs, b_wba], w=[b_pP[jj]], sig=(kc == 3))
                        TT("dve", GAt[:, g * 512:(g + 1) * 512], pP[jj][:, :], sga[j][:, g * 512:(g + 1) * 512], ALU.mult,
                           r=[b_pP[jj], b_sga[j]], w=[b_GAt])
                    fw.dma("sp", GA_d[ot * 128:(ot + 1) * 128, :], GAt[:], reads=[b_GAt])
                def p5():
                    for h in range(8):
                        TR(pQA[0:96, h * 128:(h + 1) * 128], QA[j][:, h, :], ident_bf[:], r=[b_QA[j], b_identbf], w=[b_pQA], sig=(h == 7))
                    CP("act", qtas[:].rearrange("p a b -> p (a b)"), pQA[0:96, :], r=[b_pQA], w=[b_qtas])
                    fw.dma("sp", QT_d[:, :, ot * 128:(ot + 1) * 128].rearrange("h r t -> r h t"), qtas[:], reads=[b_qtas])
                return [p1, p2, p3, p4, p5] if own else [p1]

            NORM(0)
            NORM(1)
            HEAD(0)
            for n in range(NT):
                if n + 2 < NT:
                    NORM(n + 2)
                pieces = TAIL(n)
                if n + 1 < NT:
                    HEAD(n + 1, pieces)
                else:
                    for p_ in pieces:
                        p_()
            fw.barrier()

        wst.close()
        wbb = SB(gs, "wbb", [128, 4, 1024], BF16); b_wbb = Buf()
        wo = SB(gs, "wo", [128, 8, 1024], BF16); b_wo = Buf()
        wr = SB(gs, "wr", [128, 8, 36], F32); b_wr = Buf()
        br = SB(gs, "br", [1, 36], F32); b_br = Buf()
        fw.dma("pool", wbb[:], w_bb.rearrange("(kc p) f -> p kc f", p=128), writes=[b_wbb])
        fw.dma("pool", wo[:], w_out.rearrange("(kc p) f -> p kc f", p=128), writes=[b_wo])
        fw.dma("sp", wr[:], w_rt.rearrange("(kc p) f -> p kc f", p=128), writes=[b_wr])
        fw.dma("sp", br[:], b_rt, writes=[b_br])
        attn = SB(gs, "attn", [128, NOT, 512], BF16); b_attn = [Buf() for _ in range(NOT)]

        chk(2)
        with ExitStack() as ph:
            kta = [SB(ph, "kta%d" % i, [96, 8192], BF16) for i in range(2)]; b_kta = [Buf(), Buf()]
            vsb = [SB(ph, "vsb%d" % i, [128, 64, 65], BF16) for i in range(2)]; b_vsb = [Buf(), Buf()]
            qta = [SB(ph, "qta%d" % i, [96, 4096], BF16) for i in range(2)]; b_qta = [Buf(), Buf()]
            pTt = [SB(ph, "pTt%d" % i, [128, 512], BF16) for i in range(4)]; b_pTt = [Buf() for _ in range(4)]
            rden = SB(ph, "rden", [128, 4], F32); b_rden = Buf()
            pS = [PS(ph, "pS%d" % i, [128, 512], F32) for i in range(4)]; b_pS = [PBuf() for _ in range(4)]
            pO = [PS(ph, "pO%d" % i, [128, 512], F32) for i in range(4)]; b_pO = [PBuf() for _ in range(4)]
            cv = [SB(ph, "cv%d" % i, [128, 4096], BF16) for i in range(4)]; b_cv = [Buf() for _ in range(4)]
            w_src = [w_gate.rearrange("e (p k) f -> e p (k f)", k=8), w_up.rearrange("e (p k) f -> e p (k f)", k=8),
                     w_down.rearrange("e (p k) f -> e p (k f)", k=4)]

            def conv(u):
                e_, m_ = u // 3, u % 3
                c_ = u % 4
                fw.dma("pool", cv[c_][:], w_src[m_][e_], writes=[b_cv[c_]])
                fw.dma("sp", WB_d[m_][e_ * 128:(e_ + 1) * 128, :], cv[c_][:], reads=[b_cv[c_]])

            for bfi in range(2):
                fw.dma("pool", kta[bfi][64:96, :], c_onehot, writes=[b_kta[bfi]])
                MEMSET("pool", vsb[bfi][:, :, 64:65], 1.0, w=[b_vsb[bfi]])

            def load_head(h):
                bfi = h % 2
                fw.dma("sp", kta[bfi][0:64, :], KT_d[h // 2, (h % 2) * 64:(h % 2) * 64 + 64, :], writes=[b_kta[bfi]])
                vv = V_d[:, h * 64:(h + 1) * 64].rearrange("(t p) d -> p t d", p=128)
                for t8_ in range(8):
                    fw.dma("sp", vsb[bfi][:, t8_ * 8:(t8_ + 1) * 8, 0:64], vv[:, t8_ * 8:(t8_ + 1) * 8, :], writes=[b_vsb[bfi]])
                fw.dma("sp", qta[bfi][:], QT_d[h], writes=[b_qta[bfi]])

            items = []

            def mk_gov(h, i, pr, s, O, bO, first0):
                bfi = h % 2
                K = kta[bfi]; V = vsb[bfi]; qs = qta[bfi][:, i * 256:(i + 1) * 256]
                rds = [b_kta[bfi], b_qta[bfi]]

                def qk():
                    for a in range(2):
                        kt = 2 * pr + a
                        MM(pS[s][:, a * 256:(a + 1) * 256], K[:, kt * 128:(kt + 1) * 128], qs, r=rds, w=[b_pS[s]], sig=(a == 1))

                def post():
                    ACT(pTt[s][:], pS[s][:, :], AF.Exp, r=[b_pS[s]], w=[b_pTt[s]])

                def pv():
                    for a in range(2):
                        kt = 2 * pr + a
                        for q in range(2):
                            MM(O[q][:, 0:65], pTt[s][:, a * 256 + q * 128:a * 256 + (q + 1) * 128], V[:, kt, :],
                               start=(first0 and a == 0), stop=False, r=[b_pTt[s], b_vsb[bfi]], w=[bO[q]], sig=False)
                return qk, post, pv

            def mk_diag(h, i, s, O, bO):
                bfi = h % 2
                K = kta[bfi]; V = vsb[bfi]; qs = qta[bfi][:, i * 256:(i + 1) * 256]
                rds = [b_kta[bfi], b_qta[bfi]]
                kt0 = 4 * i + 2

                def qk():
                    MM(pS[s][:, 0:256], K[:, kt0 * 128:(kt0 + 1) * 128], qs, r=rds, w=[b_pS[s]], sig=False)
                    MM(pS[s][:, 256:384], K[:, (kt0 + 1) * 128:(kt0 + 2) * 128], qs[:, 128:256], r=rds, w=[b_pS[s]])

                def post():
                    ACT(pTt[s][:, 0:384], pS[s][:, 0:384], AF.Exp, r=[b_pS[s]], w=[b_pTt[s]])
                    TT("dve", pTt[s][:, 0:128], pTt[s][:, 0:128], tri_bf[:], ALU.mult, r=[b_pTt[s], b_tri], w=[b_pTt[s]])
                    TT("pool", pTt[s][:, 256:384], pTt[s][:, 256:384], tri_bf[:], ALU.mult, r=[b_pTt[s], b_tri], w=[b_pTt[s]])

                def pv():
                    MM(O[0][:, 0:65], pTt[s][:, 0:128], V[:, kt0, :], start=False, stop=True,
                       r=[b_pTt[s], b_vsb[bfi]], w=[bO[0]])
                    MM(O[1][:, 0:65], pTt[s][:, 128:256], V[:, kt0, :], start=False, stop=False,
                       r=[b_pTt[s], b_vsb[bfi]], w=[bO[1]], sig=False)
                    MM(O[1][:, 0:65], pTt[s][:, 256:384], V[:, kt0 + 1, :], start=False, stop=True,
                       r=[b_pTt[s], b_vsb[bfi]], w=[bO[1]])
                    for q in range(2):
                        ot = 2 * i + q
                        fw.op("dve", lambda e, q=q: e.reciprocal(out=rden[:, q:q + 1], in_=O[q][:, 64:65]), [bO[q]], [b_rden])
                        TS("dve", attn[:, ot, h * 64:(h + 1) * 64], O[q][:, 0:64], rden[:, q:q + 1], None, ALU.mult,
                           r=[bO[q], b_rden], w=[b_attn[ot]])
                return qk, post, pv

            sc = 0
            oc = 0
            for h in range(8):
                for i in range(NPAIR):
                    O = [pO[(oc % 2) * 2 + q] for q in range(2)]
                    bO = [b_pO[(oc % 2) * 2 + q] for q in range(2)]
                    oc += 1
                    for pr in range(2 * i + 1):
                        it = mk_gov(h, i, pr, sc % 4, O, bO, pr == 0)
                        items.append((it, h if (i == 0 and pr == 0) else None))
                        sc += 1
                    items.append((mk_diag(h, i, sc % 4, O, bO), None))
                    sc += 1
            load_head(0)
            LA = 3
            for n in range(len(items) + LA):
                if n % 22 == 0 and n // 22 < 96:
                    conv(n // 22)
                if n < len(items):
                    items[n][0][0]()
                if n - LA >= 0:
                    (qk, post, pv), hstart = items[n - LA]
                    post()
                    pv()
                    if hstart is not None and hstart + 1 < 8:
                        load_head(hstart + 1)
            if debug:
                for ot in range(NOT):
                    fw.dma("sp", AT_d[ot * 128:(ot + 1) * 128, :], attn[:, ot, :], reads=[b_attn[ot]])
            fw.barrier()

        A_all = SB(gs, "A_all", [128, NOT, 32], F32); b_A = Buf()
        W_all = SB(gs, "W_all", [128, NOT, 32], F32); b_W = Buf()
        R_all = SB(gs, "R_all", [128, NOT, 32], F32); b_R = Buf()
        base = SB(gs, "base", [128, 32], F32); b_base = Buf()
        idx_all = SB(gs, "idx_all", [128, NOT, 2], I32); b_idx = Buf()
        wsel = SB(gs, "wsel", [128, NOT, 2], F32); b_wsel = Buf()
        widx = SB(gs, "widx", [128, NSLOT], I32); b_widx = Buf()
        b_XS = Buf(); b_Y = Buf()

        chk(3)
        with ExitStack() as ph:
            def D2(name, shape, dt):
                return [SB(ph, "%s%d" % (name, i), shape, dt) for i in range(2)], [Buf(), Buf()]
            gat, b_gat = D2("gat", [128, 1024], F32)
            gbt, b_gbt = D2("gbt", [128, 1024], F32)
            xt, b_xt = D2("xtc", [128, 1024], F32)
            aT = SB(ph, "aT", [128, 4, 128], BF16); b_aT = Buf()
            t32a = SB(ph, "t32a", [128, 1024], F32); b_t32a = Buf()
            t32b = SB(ph, "t32b", [128, 1024], F32); b_t32b = Buf()
            t32c = SB(ph, "t32c", [128, 1024], F32); b_t32c = Buf()
            mb, b_mb = D2("mb", [128, 1024], BF16)
            mT = SB(ph, "mT", [128, 8, 128], BF16); b_mT = Buf()
            x1, b_x1 = D2("x1", [128, 1024], F32)
            h2, b_h2 = D2("h2", [128, 1024], F32)
            h2b = SB(ph, "h2b", [128, 1024], BF16); b_h2b = Buf()
            h2T = SB(ph, "h2T", [128, 8, 128], F32); b_h2T = Buf()
            junk = SB(ph, "junkc", [128, 1024], BF16); b_junk = Buf()
            ss = SB(ph, "ssc", [128, 8], F32); b_ss = Buf()
            lgall = SB(ph, "lgall", [128, NOT, 36], F32); b_lgall = Buf()
            gmax = SB(ph, "gmax", [128, NOT], F32); b_gmax = Buf()
            gsum = SB(ph, "gsum", [128, NOT], F32); b_gsum = Buf()
            m1 = SB(ph, "m1", [128, NOT], F32); b_m1 = Buf()
            m2 = SB(ph, "m2", [128, NOT], F32); b_m2 = Buf()
            ohg = SB(ph, "ohg", [128, NOT, 4], F32); b_ohg = Buf()
            gtmp = SB(ph, "gtmp", [128, NOT, 4], F32); b_gtmp = Buf()
            em = SB(ph, "em", [128, NOT, 32], F32); b_em = Buf()
            eq1 = SB(ph, "eq1", [128, NOT, 32], F32); b_eq1 = Buf()
            csum = SB(ph, "csum", [128, NOT, 32], F32); b_csum = Buf()
            abf = SB(ph, "abf", [128, NOT, 32], BF16); b_abf = Buf()
            pTa = PS(ph, "pTa", [128, 1024], BF16); b_pTa = PBuf()
            pTm = PS(ph, "pTm", [128, 1024], BF16); b_pTm = PBuf()
            pP = [PS(ph, "pPc%d" % i, [128, 512], F32) for i in range(2)]; b_pP = [PBuf(), PBuf()]
            pH = [PS(ph, "pH%d" % i, [128, 512], F32) for i in range(2)]; b_pH = [PBuf(), PBuf()]
            pL = PS(ph, "pL", [128, 512], F32); b_pL = PBuf()

            MEMSET("dve", base[:], 0.0, w=[b_base])
            zt = SB(ph, "zt", [128, 4, 1024], BF16); b_zt = Buf()
            MEMSET("pool", zt[:], 0.0, w=[b_zt])
            for s_ in range(NSLOT // 2):
                fw.dma("sp", XS_d[s_ * 512:(s_ + 1) * 512, :].rearrange("(r p) f -> p r f", p=128), zt[:], reads=[b_zt])

            pc = [0]

            def load_g(ot):
                j = ot % 2
                fw.dma("act", gat[j][:], GA_d[ot * 128:(ot + 1) * 128, :], writes=[b_gat[j]])
                fw.dma("act", gbt[j][:], GB_d[ot * 128:(ot + 1) * 128, :], writes=[b_gbt[j]])

            def S1a(ot):
                j = ot % 2
                fw.dma("act", xt[j][:], x_own[ot * 128:(ot + 1) * 128, :], writes=[b_xt[j]])
                for kc in range(4):
                    TR(pTa[:, kc * 128:(kc + 1) * 128], attn[:, ot, kc * 128:(kc + 1) * 128], ident_bf[:],
                       r=[b_attn[ot], b_identbf], w=[b_pTa], sig=(kc == 3))
                CP("act", aT[:].rearrange("p a b -> p (a b)"), pTa[:, 0:512], r=[b_pTa], w=[b_aT])

            def S1b(ot):
                j = ot % 2
                for g in range(2):
                    k_ = pc[0] % 2; pc[0] += 1
                    for kc in range(4):
                        MM(pP[k_][:, :], aT[:, kc, :], wbb[:, kc, g * 512:(g + 1) * 512], start=(kc == 0), stop=(kc == 3),
                           r=[b_aT, b_wbb], w=[b_pP[k_]], sig=(kc == 3))
                    TT("dve", t32a[:, g * 512:(g + 1) * 512], pP[k_][:, :], gbt[j][:, g * 512:(g + 1) * 512], ALU.mult,
                       r=[b_pP[k_], b_gbt[j]], w=[b_t32a])
                TT("dve", mb[j][:], t32a[:], gat[j][:], ALU.add, r=[b_t32a, b_gat[j]], w=[b_mb[j]])

            def S2a(ot):
                j = ot % 2
                for kc in range(8):
                    TR(pTm[:, kc * 128:(kc + 1) * 128], mb[j][:, kc * 128:(kc + 1) * 128], ident_bf[:],
                       r=[b_mb[j], b_identbf], w=[b_pTm], sig=(kc == 7))
                CP("act", mT[:].rearrange("p a b -> p (a b)"), pTm[:, :], r=[b_pTm], w=[b_mT])

            def S2b(ot):
                j = ot % 2
                for g in range(2):
                    k_ = pc[0] % 2; pc[0] += 1
                    for kc in range(8):
                        MM(pP[k_][:, :], mT[:, kc, :], wo[:, kc, g * 512:(g + 1) * 512], start=(kc == 0), stop=(kc == 7),
                           r=[b_mT, b_wo], w=[b_pP[k_]], sig=(kc == 7))
                    TT("dve", t32b[:, g * 512:(g + 1) * 512], pP[k_][:, :], GT1[:, g * 512:(g + 1) * 512], ALU.mult,
                       r=[b_pP[k_], b_mod], w=[b_t32b])
                TT("pool", x1[j][:], t32b[:], xt[j][:], ALU.add, r=[b_t32b, b_xt[j]], w=[b_x1[j]])
                fw.dma("sp", X1_d[ot * 128:(ot + 1) * 128, :], x1[j][:], reads=[b_x1[j]])

            def S3(ot):
                j = ot % 2
                RSTD(x1[j][:], b_x1[j], ss, b_ss, 1024)
                STT(t32c[:], x1[j][:], ss[:, 2:3], G2, ALU.mult, ALU.mult, r=[b_x1[j], b_ss, b_mod], w=[b_t32c])
                TT("pool", h2[j][:], t32c[:], SH2, ALU.add, r=[b_t32c, b_mod], w=[b_h2[j]])
                CP("act", h2b[:], h2[j][:], r=[b_h2[j]], w=[b_h2b])
                fw.dma("sp", H2_d[ot * 128:(ot + 1) * 128, :], h2b[:], reads=[b_h2b])

            def S4a(ot):
                j = ot % 2
                for kc in range(8):
                    TR(pH[kc // 4][:, (kc % 4) * 128:(kc % 4 + 1) * 128], h2[j][:, kc * 128:(kc + 1) * 128], ident_f[:],
                       r=[b_h2[j], b_identf], w=[b_pH[kc // 4]], sig=(kc % 4 == 3))
                for hh in range(2):
                    CP("act" if hh == 0 else "dve", h2T[:, hh * 4:(hh + 1) * 4, :].rearrange("p a b -> p (a b)"), pH[hh][:, :],
                       r=[b_pH[hh]], w=[b_h2T])

            def S4b(ot):
                j = ot % 2
                for kc in range(8):
                    MM(pL[:, 0:36], h2T[:, kc, :], wr[:, kc, :], start=(kc == 0), stop=False, r=[b_h2T, b_wr], w=[b_pL], sig=False)
                MM(pL[:, 0:36], ones_f[0:1, :], br[0:1, :], start=False, stop=True, r=[b_onesf, b_br], w=[b_pL])
                CP("act", lgall[:, ot, :], pL[:, 0:36], r=[b_pL], w=[b_lgall])

            load_g(0)
            for k in range(NOT + 3):
                def ok(t):
                    return 0 <= t < NOT
                if ok(k + 1):
                    load_g(k + 1)
                if ok(k): S1a(k)
                if ok(k - 1): S2a(k - 1)
                if ok(k - 3): S4a(k - 3)
                if ok(k): S1b(k)
                if ok(k - 1): S2b(k - 1)
                if ok(k - 2): S3(k - 2)
                if ok(k - 3): S4b(k - 3)
            T = NOT
            G = lgall[:, :, 0:4]
            E4 = lgall[:, :, 4:36].rearrange("p t (g e) -> p t g e", g=4)
            bc3 = lambda a, n: a.unsqueeze(2).to_broadcast([128, T, n])
            RED(gmax[:], G, ALU.max, r=[b_lgall], w=[b_gmax])
            TT("dve", ohg[:], G, bc3(gmax[:], 4), ALU.is_equal, r=[b_lgall, b_gmax], w=[b_ohg])
            TT("dve", gtmp[:], G, bc3(gmax[:], 4), ALU.subtract, r=[b_lgall, b_gmax], w=[b_gtmp])
            ACT(gtmp[:], gtmp[:], AF.Exp, r=[b_gtmp], w=[b_gtmp])
            RED(gsum[:], gtmp[:], ALU.add, r=[b_gtmp], w=[b_gsum])
            fw.op("dve", lambda e: e.reciprocal(out=gsum[:], in_=gsum[:]), [b_gsum], [b_gsum])
            TS("dve", ohg[:], ohg[:], -1.0, 1e30, ALU.add, ALU.mult, r=[b_ohg], w=[b_ohg])
            TT("dve", em[:].rearrange("p t (g e) -> p t g e", g=4), E4,
               ohg[:].unsqueeze(3).to_broadcast([128, T, 4, 8]), ALU.add, r=[b_lgall, b_ohg], w=[b_em])
            RED(m1[:], em[:], ALU.max, r=[b_em], w=[b_m1])
            TT("dve", eq1[:], em[:], bc3(m1[:], 32), ALU.is_equal, r=[b_em, b_m1], w=[b_eq1])
            STT(eq1[:].rearrange("p a b -> p (a b)"), eq1[:].rearrange("p a b -> p (a b)"), -1e30,
                em[:].rearrange("p a b -> p (a b)"), ALU.mult, ALU.add, r=[b_eq1, b_em], w=[b_eq1])
            RED(m2[:], eq1[:], ALU.max, r=[b_eq1], w=[b_m2])
            TT("dve", A_all[:], em[:], bc3(m2[:], 32), ALU.is_ge, r=[b_em, b_m2], w=[b_A])
            TT("dve", eq1[:], em[:], bc3(m1[:], 32), ALU.subtract, r=[b_em, b_m1], w=[b_eq1])
            ACT(eq1[:], eq1[:], AF.Exp, r=[b_eq1], w=[b_eq1])
            TT("dve", eq1[:], eq1[:], A_all[:], ALU.mult, r=[b_eq1, b_A], w=[b_eq1])
            RED(m1[:], eq1[:], ALU.add, r=[b_eq1], w=[b_m1])
            fw.op("dve", lambda e: e.reciprocal(out=m1[:], in_=m1[:]), [b_m1], [b_m1])
            TT("dve", m1[:], m1[:], gsum[:], ALU.mult, r=[b_m1, b_gsum], w=[b_m1])
            TT("dve", W_all[:], eq1[:], bc3(m1[:], 32), ALU.mult, r=[b_eq1, b_m1], w=[b_W])
            CP("dve", abf[:], A_all[:], r=[b_A], w=[b_abf])
            abf2 = abf[:].rearrange("p a b -> p (a b)")
            for hh in range(2):
                MM(pH[hh][:, :], tris_bf[:], abf2[:, hh * 512:(hh + 1) * 512], r=[b_tris, b_abf], w=[b_pH[hh]])
                MM(pP[hh][:, :], ones_bf[:], abf2[:, hh * 512:(hh + 1) * 512], r=[b_onesbf, b_abf], w=[b_pP[hh]])
            cs = [eq1, em]; b_cs = [b_eq1, b_em]
            for hh in range(2):
                CP("act", cs[0][:, hh * 16:(hh + 1) * 16, :].rearrange("p a b -> p (a b)"), pP[hh][:, :], r=[b_pP[hh]], w=[b_cs[0]])
            CP("dve", csum[:], cs[0][:], r=[b_cs[0]], w=[b_csum])
            cur = 0
            for sh in (1, 2, 4, 8, 16):
                nx = 1 - cur
                CP("dve", cs[nx][:, 0:sh, :], cs[cur][:, 0:sh, :], r=[b_cs[cur]], w=[b_cs[nx]])
                TT("dve", cs[nx][:, sh:T, :], cs[cur][:, sh:T, :], cs[cur][:, 0:T - sh, :], ALU.add, r=[b_cs[cur]], w=[b_cs[nx]])
                cur = nx
            CP("dve", base[:], cs[cur][:, T - 1, :], r=[b_cs[cur]], w=[b_base])
            TT("dve", csum[:], cs[cur][:], csum[:], ALU.subtract, r=[b_cs[cur], b_csum], w=[b_csum])
            for hh in range(2):
                TT("dve", R_all[:, hh * 16:(hh + 1) * 16, :].rearrange("p a b -> p (a b)"), pH[hh][:, :],
                   csum[:, hh * 16:(hh + 1) * 16, :].rearrange("p a b -> p (a b)"), ALU.add, r=[b_pH[hh], b_csum], w=[b_R])
            fw.barrier()

        chk(4)
        with ExitStack() as ph:
            nb = SB(ph, "nb", [128, 32], F32); b_nb = Buf()
            cmp3 = SB(ph, "cmp3", [128, 64, 32], F32); b_cmp3 = Buf()
            inc = [SB(ph, "inc%d" % i, [128, 32], F32) for i in range(2)]; b_inc = [Buf(), Buf()]
            pst = SB(ph, "pst", [128, 32], F32); b_pst = Buf()
            bef = SB(ph, "bef", [128, 64], F32); b_bef = Buf()
            trail = SB(ph, "trail", [128, 64], F32); b_trail = Buf()
            ddb = SB(ph, "ddb", [128, NOT, 32], F32); b_ddb = Buf()
            d1b = SB(ph, "d1b", [128, NOT, 32], F32); b_d1b = Buf()
            d2b = SB(ph, "d2b", [128, NOT, 32], F32); b_d2b = Buf()
            mxn = SB(ph, "mxn", [128, 3, NOT], F32); b_mxn = Buf()
            idxf = SB(ph, "idxf", [128, NOT, 2], F32); b_idxf = Buf()
            h2l = [SB(ph, "h2l%d" % i, [128, 1024], BF16) for i in range(4)]; b_h2l = [Buf() for _ in range(4)]
            TT("dve", cmp3[:, 0:32, 0:16], base[:].unsqueeze(2).to_broadcast([128, 32, 16]),
               misc[:, 0:16].unsqueeze(1).to_broadcast([128, 32, 16]), ALU.is_gt, r=[b_base, b_misc], w=[b_cmp3])
            RED(nb[:], cmp3[:, 0:32, 0:16], ALU.add, r=[b_cmp3], w=[b_nb])
            CP("dve", inc[0][:], nb[:], r=[b_nb], w=[b_inc[0]])
            cur = 0
            for sh in (1, 2, 4, 8, 16):
                nx = 1 - cur
                CP("dve", inc[nx][:, 0:sh], inc[cur][:, 0:sh], r=[b_inc[cur]], w=[b_inc[nx]])
                TT("dve", inc[nx][:, sh:32], inc[cur][:, sh:32], inc[cur][:, 0:32 - sh], ALU.add, r=[b_inc[cur]], w=[b_inc[nx]])
                cur = nx
            incl = inc[cur]; b_incl = b_inc[cur]
            TT("dve", pst[:], incl[:], nb[:], ALU.subtract, r=[b_incl, b_nb], w=[b_pst])
            TS("dve", pst[:], pst[:], 256.0, None, ALU.mult, r=[b_pst], w=[b_pst])
            TT("dve", cmp3[:], incl[:].unsqueeze(1).to_broadcast([128, 64, 32]),
               misc[:, 16:80].unsqueeze(2).to_broadcast([128, 64, 32]), ALU.is_le, r=[b_incl, b_misc], w=[b_cmp3])
            RED(bef[:], cmp3[:], ALU.add, r=[b_cmp3], w=[b_bef])
            TS("dve", bef[:], bef[:], 31.0, 128.0, ALU.min, ALU.mult, r=[b_bef], w=[b_bef])
            TS("dve", bef[:], bef[:], misc[:, 80:81], None, ALU.add, r=[b_bef, b_misc], w=[b_bef])
            TS("dve", trail[:], misc[:, 16:80], incl[:, 31:32], 4096.0, ALU.is_ge, ALU.mult, r=[b_misc, b_incl], w=[b_trail])
            TT("dve", bef[:], bef[:], trail[:], ALU.add, r=[b_bef, b_trail], w=[b_bef])
            CP("dve", widx[:], bef[:], r=[b_bef], w=[b_widx])
            R3 = R_all[:]; A3 = A_all[:]; W3 = W_all[:]
            flat = lambda t: t[:].rearrange("p a b -> p (a b)")
            TT("dve", ddb[:], R3, pst[:].unsqueeze(1).to_broadcast([128, NOT, 32]), ALU.add, r=[b_R, b_pst], w=[b_ddb])
            STT(flat(d1b), flat(ddb), 1.0, flat(A_all), ALU.add, ALU.mult, r=[b_ddb, b_A], w=[b_d1b])
            STT(flat(d2b), flat(ddb), -1048576.0, flat(A_all), ALU.add, ALU.mult, r=[b_ddb, b_A], w=[b_d2b])
            RED(mxn[:, 0, :], d1b[:], ALU.max, r=[b_d1b], w=[b_mxn])
            RED(mxn[:, 1, :], d2b[:], ALU.min, r=[b_d2b], w=[b_mxn])
            TS("dve", idxf[:, :, 0], mxn[:, 1, :], 1048576.0, None, ALU.add, r=[b_mxn], w=[b_idxf])
            TS("dve", idxf[:, :, 1], mxn[:, 0, :], -1.0, None, ALU.add, r=[b_mxn], w=[b_idxf])
            CP("dve", flat(idx_all), flat(idxf), r=[b_idxf], w=[b_idx])
            TT("dve", d2b[:], d1b[:], mxn[:, 0, :].unsqueeze(2).to_broadcast([128, NOT, 32]), ALU.is_equal, r=[b_d1b, b_mxn], w=[b_d2b])
            TT("dve", d2b[:], d2b[:], W3, ALU.mult, r=[b_d2b, b_W], w=[b_d2b])
            RED(wsel[:, :, 1], d2b[:], ALU.add, r=[b_d2b], w=[b_wsel])
            RED(mxn[:, 2, :], W3, ALU.add, r=[b_W], w=[b_mxn])
            TT("dve", wsel[:, :, 0], mxn[:, 2, :], wsel[:, :, 1], ALU.subtract, r=[b_mxn, b_wsel], w=[b_wsel])
            NHB = 4
            for ot in range(min(NHB - 1, NOT)):
                fw.dma("sp", h2l[ot % NHB][:], H2_d[ot * 128:(ot + 1) * 128, :], writes=[b_h2l[ot % NHB]])
            for ot in range(NOT):
                j = ot % NHB
                nx = ot + NHB - 1
                if nx < NOT:
                    fw.dma("sp", h2l[nx % NHB][:], H2_d[nx * 128:(nx + 1) * 128, :], writes=[b_h2l[nx % NHB]])
                for k_ in range(2):
                    fw.dma("pool", XS_d[:, :], h2l[j][:], reads=[b_h2l[j], b_idx],
                           indirect=dict(out_offset=bass.IndirectOffsetOnAxis(ap=idx_all[:, ot, k_:k_ + 1], axis=0), in_offset=None))
            if debug:
                rt = SB(ph, "rt", [128, 8], F32); b_rtb = Buf()
                for ot in range(NOT):
                    CP("dve", rt[:, 0:2], idx_all[:, ot, :], r=[b_idx], w=[b_rtb])
                    CP("dve", rt[:, 2:4], wsel[:, ot, :], r=[b_wsel], w=[b_rtb])
                    CP("dve", rt[:, 4:8], widx[:, ot * 2:ot * 2 + 4] if ot < 30 else widx[:, 60:64], r=[b_widx], w=[b_rtb])
                    fw.dma("sp", RT_d[ot * 128:(ot + 1) * 128, :], rt[:], reads=[b_rtb])
            fw.barrier()

        chk(5)
        with ExitStack() as ph:
            wg = [SB(ph, "wg%d" % i, [128, 8, 512], BF16) for i in range(2)]
            wu = [SB(ph, "wu%d" % i, [128, 8, 512], BF16) for i in range(2)]
            wd = [SB(ph, "wd%d" % i, [128, 4, 1024], BF16) for i in range(2)]
            b_wg = [Buf(), Buf()]; b_wu = [Buf(), Buf()]; b_wd = [Buf(), Buf()]
            xs = [SB(ph, "xs%d" % i, [128, 2, 1024], BF16) for i in range(2)]; b_xs = [Buf(), Buf()]
            xT = [SB(ph, "xT%d" % i, [128, 8, 256], BF16) for i in range(2)]; b_xT = [Buf(), Buf()]
            sg = [SB(ph, "sg%d" % i, [128, 256], F32) for i in range(4)]; b_sg = [Buf() for _ in range(4)]
            hidT = [SB(ph, "hidT%d" % i, [128, 4, 256], BF16) for i in range(2)]; b_hidT = [Buf(), Buf()]
            ysb = [SB(ph, "ysb%d" % i, [128, 1024], F32) for i in range(2)]; b_ysb = [Buf(), Buf()]
            pX = [PS(ph, "pX%d" % i, [128, 1024], BF16) for i in range(2)]; b_pX = [PBuf(), PBuf()]
            pG = [PS(ph, "pG%d" % i, [128, 512], F32) for i in range(4)]; b_pG = [PBuf() for _ in range(4)]
            pY = [PS(ph, "pY%d" % i, [128, 512], F32) for i in range(2)]; b_pY = [PBuf(), PBuf()]
            wgv, wuv, wdv = WB_d[0][0:4096, :], WB_d[1][0:4096, :], WB_d[2][0:4096, :]

            bc_reg = nc.gpsimd.to_reg(4095)

            def load_slot(s):
                j = s % 2
                off = dict(out_offset=None, in_offset=bass.IndirectOffsetOnAxis(ap=widx[:, s:s + 1], axis=0),
                           bounds_check=bc_reg, oob_is_err=False)
                fw.dma("pool", wg[j][:].rearrange("p a b -> p (a b)"), wgv, reads=[b_widx], writes=[b_wg[j]], indirect=off)
                fw.dma("pool", wu[j][:].rearrange("p a b -> p (a b)"), wuv, reads=[b_widx], writes=[b_wu[j]], indirect=off)
                fw.dma("pool", wd[j][:].rearrange("p a b -> p (a b)"), wdv, reads=[b_widx], writes=[b_wd[j]], indirect=off)
                fw.dma("act", xs[j][:], XS_d[s * 256:(s + 1) * 256, :].rearrange("(r p) f -> p r f", p=128),
                       writes=[b_xs[j]])

            def XT(s):
                j = s % 2
                for r_ in range(2):
                    xv = xs[j][:, r_, :].rearrange("p (a k) -> p k a", k=8)
                    for kc in range(8):
                        TR(pX[r_][:, kc * 128:(kc + 1) * 128], xv[:, kc, :], ident_bf[:], r=[b_xs[j], b_identbf], w=[b_pX[r_]], sig=(kc == 7))
                    CP("act" if r_ == 0 else "dve", xT[j][:, :, r_ * 128:(r_ + 1) * 128],
                       pX[r_][:, :].rearrange("p (a b) -> p a b", a=8), r=[b_pX[r_]], w=[b_xT[j]])

            def GU(s):
                j = s % 2
                wgs = wg[j][:].rearrange("p k (m c) -> p k c m", c=4)
                wus = wu[j][:].rearrange("p k (m c) -> p k c m", c=4)
                for hc in range(4):
                    g_ = hc
                    for kc in range(8):
                        MM(pG[g_][:, 0:256], wgs[:, kc, hc, :], xT[j][:, kc, :], start=(kc == 0), stop=(kc == 7),
                           r=[b_wg[j], b_xT[j]], w=[b_pG[g_]], sig=False)
                    for kc in range(8):
                        MM(pG[g_][:, 256:512], wus[:, kc, hc, :], xT[j][:, kc, :], start=(kc == 0), stop=(kc == 7),
                           r=[b_wu[j], b_xT[j]], w=[b_pG[g_]], sig=(kc == 7))
                    ACT(sg[g_][:], pG[g_][:, 0:256], AF.Silu, r=[b_pG[g_]], w=[b_sg[g_]])
                    TT("dve", hidT[j][:, hc, :], sg[g_][:], pG[g_][:, 256:512], ALU.mult, r=[b_sg[g_], b_pG[g_]], w=[b_hidT[j]])

            yc = [0]

            def DOWN(s):
                j = s % 2
                for r_ in range(2):
                    yb = yc[0] % 2; yc[0] += 1
                    for cg in range(2):
                        for hc in range(4):
                            MM(pY[cg][:, :], hidT[j][:, hc, r_ * 128:(r_ + 1) * 128], wd[j][:, hc, cg * 512:(cg + 1) * 512],
                               start=(hc == 0), stop=(hc == 3), r=[b_hidT[j], b_wd[j]], w=[b_pY[cg]], sig=(hc == 3))
                        CP("act" if cg == 0 else "dve", ysb[yb][:, cg * 512:(cg + 1) * 512], pY[cg][:, :], r=[b_pY[cg]], w=[b_ysb[yb]])
                    row = s * 256 + r_ * 128
                    fw.dma("sp", Y_d[row:row + 128, :], ysb[yb][:], reads=[b_ysb[yb]])

            load_slot(0)
            load_slot(1)
            XT(0)
            for s in range(NSLOT):
                GU(s)
                if s + 1 < NSLOT:
                    XT(s + 1)
                DOWN(s)
                if s + 2 < NSLOT:
                    load_slot(s + 2)
            fw.barrier()

        chk(6)
        with ExitStack() as ph:
            x1t = [SB(ph, "x1t%d" % i, [128, 1024], F32) for i in range(4)]; b_x1t = [Buf() for _ in range(4)]
            ylo = [SB(ph, "ylo%d" % i, [128, 1024], F32) for i in range(2)]; b_ylo = [Buf(), Buf()]
            yhi = [SB(ph, "yhi%d" % i, [128, 1024], F32) for i in range(2)]; b_yhi = [Buf(), Buf()]
            t1 = [SB(ph, "t1_%d" % i, [128, 1024], F32) for i in range(2)]; b_t1 = [Buf(), Buf()]
            t2 = [SB(ph, "t2_%d" % i, [128, 1024], F32) for i in range(2)]; b_t2 = [Buf(), Buf()]
            t3 = [SB(ph, "t3_%d" % i, [128, 1024], F32) for i in range(2)]; b_t3 = [Buf(), Buf()]
            ssf = [SB(ph, "ssf%d" % i, [128, 8], F32) for i in range(2)]; b_ssf = [Buf(), Buf()]
            xo = [SB(ph, "xo%d" % i, [128, 1024], F32) for i in range(4)]; b_xo = [Buf() for _ in range(4)]
            junk = SB(ph, "junkf", [128, 1024], BF16); b_junk = Buf()
            ss = SB(ph, "ssf", [128, 8], F32); b_ss = Buf()
            ob = [SB(ph, "ob%d" % i, [128, 1024], F32) for i in range(2)]; b_ob = [Buf(), Buf()]
            b_out = Buf()

            def load_f(ot):
                j = ot % 2
                fw.dma("sp", x1t[ot % 4][:], X1_d[ot * 128:(ot + 1) * 128, :], writes=[b_x1t[ot % 4]])
                fw.dma("pool", ylo[j][:], Y_d[:, :], reads=[b_idx], writes=[b_ylo[j]],
                       indirect=dict(out_offset=None, in_offset=bass.IndirectOffsetOnAxis(ap=idx_all[:, ot, 0:1], axis=0)))
                fw.dma("pool", yhi[j][:], Y_d[:, :], reads=[b_idx], writes=[b_yhi[j]],
                       indirect=dict(out_offset=None, in_offset=bass.IndirectOffsetOnAxis(ap=idx_all[:, ot, 1:2], axis=0)))

            def Fa(ot):
                j = ot % 2
                ACT(t1[j][:], ylo[j][:], AF.Copy, r=[b_ylo[j], b_wsel], w=[b_t1[j]], scale=wsel[:, ot, 0:1])
                STT(t2[j][:], yhi[j][:], wsel[:, ot, 1:2], t1[j][:], ALU.mult, ALU.add, r=[b_yhi[j], b_wsel, b_t1[j]], w=[b_t2[j]])

            def Fb(ot):
                j = ot % 2; q = ot % 4
                TT("dve", t3[j][:], t2[j][:], GT2, ALU.mult, r=[b_t2[j], b_mod], w=[b_t3[j]])
                TT("dve", xo[q][:], t3[j][:], x1t[q][:], ALU.add, r=[b_t3[j], b_x1t[q]], w=[b_xo[q]])

            def Fc(ot):
                j = ot % 2; q = ot % 4
                ACT(junk[:], xo[q][:], AF.Square, r=[b_xo[q]], w=[b_junk, b_ssf[j]], accum_out=ssf[j][:, 0:1])
                TS("dve", ssf[j][:, 1:2], ssf[j][:, 0:1], 1.0 / 1024, 1e-6, ALU.mult, ALU.add, r=[b_ssf[j]], w=[b_ssf[j]])
                TT("pool", ssf[j][:, 2:3], ssf[j][:, 1:2], misc[:, 81:82], ALU.pow, r=[b_ssf[j], b_misc], w=[b_ssf[j]])

            def Fd(ot):
                j = ot % 2; q = ot % 4
                STT(ob[j][:], xo[q][:], ssf[j][:, 2:3], gfin[:], ALU.mult, ALU.mult, r=[b_xo[q], b_ssf[j], b_gfin], w=[b_ob[j]])
                fw.dma("sp", out[ot * 128:(ot + 1) * 128, :], ob[j][:], reads=[b_ob[j]], writes=[b_out])

            load_f(0)
            for k in range(NOT + 3):
                if k + 1 < NOT:
                    load_f(k + 1)
                if 0 <= k < NOT: Fa(k)
                if 0 <= k - 1 < NOT: Fb(k - 1)
                if 0 <= k - 2 < NOT: Fc(k - 2)
                if 0 <= k - 3 < NOT: Fd(k - 3)
            fw.barrier()
      except _Stop:
        pass
      stats = {k: (e.n_instr, e.cnt) for k, e in gs_fw[0].engs.items()}
    return nc, stats


def _own_chunks(par):
    own, oth = [], []
    for i in range(NPAIR):
        a, b = 2 * i, 2 * i + 1
        if (i % 2 == 0) == (par == 0):
            own.append(a); oth.append(b)
        else:
            own.append(b); oth.append(a)
    return own, oth


def _rot_table():
    half = 8
    inv = np.power(np.float32(500000.0), -np.arange(half, dtype=np.float32) * np.float32(2.0) / np.float32(16)).astype(np.float32)
    ang = (np.arange(8192, dtype=np.float32)[:, None] * inv[None, :]).astype(np.float32)
    cos = np.cos(ang).astype(np.float32)
    sin = np.sin(ang).astype(np.float32)
    return np.concatenate([np.tile(cos, (1, 8)), np.tile(sin, (1, 8))], axis=1).astype(np.float32)


def make_in_maps(inputs):
    f = lambda a: np.ascontiguousarray(np.asarray(a, dtype=np.float32))
    x = f(inputs["x"]); c = f(inputs["c"])
    rot = _rot_table()
    ident = np.eye(128, dtype=np.float32)
    kk = np.arange(128)
    tri = (kk[:, None] <= kk[None, :]).astype(np.float32)
    tris = (kk[:, None] < kk[None, :]).astype(np.float32)
    onehot = (np.arange(8192)[None, :] // 256 == np.arange(32)[:, None]).astype(np.float32)
    misc = np.zeros((128, 96), np.float32)
    misc[:, 0:16] = 256.0 * np.arange(16)[None, :]
    misc[:, 16:80] = np.arange(64)[None, :]
    misc[:, 80] = np.arange(128)
    misc[:, 81] = -0.5
    w_rt = np.concatenate([f(inputs["w_router_group"])[0], f(inputs["w_router_expert"])[0].reshape(1024, 32)], axis=1)
    b_rt = np.concatenate([f(inputs["b_router_group"])[0], f(inputs["b_router_expert"])[0].reshape(32)])[None, :]
    shared = dict(
        ada_w=f(inputs["ada_w"])[0], ada_b=f(inputs["ada_b"]), norm1_g=f(inputs["norm1_g"]), norm2_g=f(inputs["norm2_g"]),
        final_g=f(inputs["final_norm_g"])[None, :], w_in=f(inputs["w_in"])[0], ln_g=f(inputs["gmlp_ln_g"]),
        ln_b=f(inputs["gmlp_ln_b"]), w_sp=f(inputs["w_spatial"])[0], b_spT=np.ascontiguousarray(f(inputs["b_spatial"])[0].T),
        w_ba=f(inputs["w_branch_a"])[0], w_bb=f(inputs["w_branch_b"])[0], w_out=f(inputs["w_out"])[0],
        w_rt=np.ascontiguousarray(w_rt), b_rt=np.ascontiguousarray(b_rt),
        w_gate=f(inputs["w_gate"])[0], w_up=f(inputs["w_up"])[0], w_down=f(inputs["w_down"])[0],
        c_ident=ident, c_tri=tri, c_tris=tris, c_onehot=onehot, c_misc=misc)
    maps, rowmaps = [], []
    for core in range(8):
        b, par = core // 2, core % 2
        own, oth = _own_chunks(par)
        rows_own = np.concatenate([np.arange(ch * 256, (ch + 1) * 256) for ch in own])
        rows_oth = np.concatenate([np.arange(ch * 256, (ch + 1) * 256) for ch in oth])
        pb_ = np.full((16, 32), -1e30, np.float32)
        for i in range(NPAIR):
            pb_[i, 0:2 * i] = 0.0
            if own[i] > oth[i]:
                pb_[i, 2 * i] = 0.0
        m = dict(shared)
        m.update(x_own=np.ascontiguousarray(x[b][rows_own]), x_oth=np.ascontiguousarray(x[b][rows_oth]),
                 rot_own=np.ascontiguousarray(rot[rows_own]), rot_oth=np.ascontiguousarray(rot[rows_oth]),
                 pastb=pb_.reshape(1, 512), c_pk=np.ascontiguousarray(c[b].reshape(8, 128).T))
        maps.append(m)
        rowmaps.append((b, rows_own))
    return maps, rowmaps


_CACHE = {}


def kernel(**inputs):
    if "nc" not in _CACHE:
        _CACHE["nc"] = build(False)[0]
    nc = _CACHE["nc"]
    maps, rowmaps = make_in_maps(inputs)
    res = run_bass_kernel_spmd(nc, maps, core_ids=list(range(8)))
    outp = np.empty((4, 8192, 1024), np.float32)
    for core in range(8):
        b, rows = rowmaps[core]
        outp[b, rows] = np.asarray(res.results[core]["out"], dtype=np.float32)
    return outp
```

```python
import numpy as np
from contextlib import ExitStack
import concourse.bass as bass
import concourse.mybir as mybir
from concourse.bass_utils import run_bass_kernel_spmd

F32 = mybir.dt.float32
BF16 = mybir.dt.bfloat16
I32 = mybir.dt.int32
AF = mybir.ActivationFunctionType
ALU = mybir.AluOpType
AX = mybir.AxisListType

NPAIR = 16
NOT = 32
NSLOT = 64
NEG = -30000.0


class Buf:
    __slots__ = ("last_w", "readers")

    def __init__(self):
        self.last_w = None
        self.readers = []


class PBuf(Buf):
    __slots__ = ("acc",)

    def __init__(self):
        super().__init__()
        self.acc = {}


class Eng:
    def __init__(self, name, h, sem, in_order=False):
        self.name, self.h, self.sem, self.in_order = name, h, sem, in_order
        self.cnt = 0
        self.waited = {}
        self.pending = False
        self.n_instr = 0


class FW:
    def __init__(self, nc, stack, n_dma_sems=48):
        self.nc = nc
        self.engs = {}
        for name, h, io in (("pe", nc.tensor, True), ("act", nc.scalar, False),
                            ("dve", nc.vector, False), ("pool", nc.gpsimd, False),
                            ("sp", nc.sync, False)):
            sem = stack.enter_context(nc.semaphore("s_" + name))
            self.engs[name] = Eng(name, h, sem, io)
        self.rings = {"sp": [], "pool": [], "act": []}
        for q, n in (("sp", 32), ("pool", 24), ("act", 12)):
            for i in range(n):
                sem = stack.enter_context(nc.semaphore("d%s%d" % (q, i)))
                self.rings[q].append([sem, 0])
        self.dma_ring = self.rings["sp"] + self.rings["pool"] + self.rings["act"]
        self.dma_i = {"sp": 0, "pool": 0, "act": 0}

    def _deps(self, eng, reads, writes):
        deps = {}

        def add(ev):
            if ev is None:
                return
            sem, val = ev
            if sem is eng.sem and eng.in_order:
                return
            k = id(sem)
            if k not in deps or deps[k][1] < val:
                deps[k] = (sem, val)
        for b in reads:
            add(b.last_w)
            if isinstance(b, PBuf):
                for nm, ev in b.acc.items():
                    if nm != eng.name:
                        add(ev)
        for b in writes:
            add(b.last_w)
            for r in b.readers:
                add(r)
            if isinstance(b, PBuf):
                for nm, ev in b.acc.items():
                    if nm != eng.name:
                        add(ev)
        return deps

    def _wait(self, eng, deps):
        for k, (sem, val) in deps.items():
            if eng.waited.get(k, 0) < val:
                eng.h.wait_ge(sem, val)
                eng.waited[k] = val
                eng.n_instr += 1

    def _update(self, ev, reads, writes, engname=None):
        for b in writes:
            b.last_w = ev
            b.readers = []
            if isinstance(b, PBuf):
                b.acc[engname] = ev
        for b in reads:
            if b in writes:
                continue
            if isinstance(b, PBuf):
                b.acc[engname] = ev
                continue
            b.readers.append(ev)
            if len(b.readers) > 48:
                best = {}
                for (s, v) in b.readers:
                    k = id(s)
                    if k not in best or best[k][1] < v:
                        best[k] = (s, v)
                b.readers = list(best.values())

    def op(self, engname, fn, reads=(), writes=(), signal=True):
        eng = self.engs[engname]
        self._wait(eng, self._deps(eng, reads, writes))
        ins = fn(eng.h)
        eng.n_instr += 1
        if signal:
            ins.then_inc(eng.sem, 1)
            eng.cnt += 1
            ev = (eng.sem, eng.cnt)
            eng.pending = False
        else:
            ev = (eng.sem, eng.cnt + 1)
            eng.pending = True
        self._update(ev, reads, writes, engname)

    def dma(self, qname, out, in_, reads=(), writes=(), indirect=None):
        eng = self.engs[qname]
        deps = self._deps(eng, reads, writes)
        ring = self.rings[qname]
        slot = ring[self.dma_i[qname] % len(ring)]
        self.dma_i[qname] += 1
        if slot[1] > 0:
            k = id(slot[0])
            if k not in deps or deps[k][1] < slot[1]:
                deps[k] = (slot[0], slot[1])
        self._wait(eng, deps)
        if indirect is None:
            ins = eng.h.dma_start(out=out, in_=in_)
        else:
            ins = eng.h.indirect_dma_start(out=out, in_=in_, **indirect)
        eng.n_instr += 1
        slot[1] += 16
        ins.then_inc(slot[0], 16)
        self._update((slot[0], slot[1]), reads, writes)

    def barrier(self):
        assert not self.engs["pe"].pending
        for e in self.engs.values():
            deps = {}
            for f in self.engs.values():
                if f is not e and f.cnt > 0:
                    deps[id(f.sem)] = (f.sem, f.cnt)
            for s in self.dma_ring:
                if s[1] > 0:
                    deps[id(s[0])] = (s[0], s[1])
            self._wait(e, deps)


class _Stop(Exception):
    pass


def build(debug=False, upto=99):
    nc = bass.Bass("TRN2", target_bir_lowering=False)

    def din(name, shape, dt=F32):
        return nc.dram_tensor(name, list(shape), dt, kind="ExternalInput").ap()

    skind = "ExternalOutput" if debug else "Internal"

    def dscr(name, shape, dt):
        return nc.dram_tensor(name, list(shape), dt, kind=skind).ap()

    x_own = din("x_own", [4096, 1024])
    x_oth = din("x_oth", [4096, 1024])
    rot_own = din("rot_own", [4096, 128])
    rot_oth = din("rot_oth", [4096, 128])
    pastb = din("pastb", [1, 512])
    c_pk = din("c_pk", [128, 8])
    ada_w = din("ada_w", [1024, 6144])
    ada_b = din("ada_b", [1, 6144])
    norm1_g = din("norm1_g", [1, 1024])
    norm2_g = din("norm2_g", [1, 1024])
    final_g = din("final_g", [1, 1024])
    w_in = din("w_in", [1024, 4608])
    ln_g = din("ln_g", [1, 512])
    ln_b = din("ln_b", [1, 512])
    w_sp = din("w_sp", [4, 128, 128])
    b_spT = din("b_spT", [128, 4])
    w_ba = din("w_ba", [512, 1024])
    w_bb = din("w_bb", [512, 1024])
    w_out = din("w_out", [1024, 1024])
    w_rt = din("w_rt", [1024, 36])
    b_rt = din("b_rt", [1, 36])
    w_gate = din("w_gate", [32, 1024, 512])
    w_up = din("w_up", [32, 1024, 512])
    w_down = din("w_down", [32, 512, 1024])
    c_ident = din("c_ident", [128, 128])
    c_tri = din("c_tri", [128, 128])
    c_tris = din("c_tris", [128, 128])
    c_onehot = din("c_onehot", [32, 8192])
    c_misc = din("c_misc", [128, 96])
    out = nc.dram_tensor("out", [4096, 1024], F32, kind="ExternalOutput").ap()

    KT_d = dscr("KT_d", [4, 128, 8192], BF16)
    V_d = dscr("V_d", [8192, 512], BF16)
    QT_d = dscr("QT_d", [8, 96, 4096], BF16)
    GA_d = dscr("GA_d", [4096, 1024], F32)
    GB_d = dscr("GB_d", [4096, 1024], F32)
    X1_d = dscr("X1_d", [4096, 1024], F32)
    H2_d = dscr("H2_d", [4096, 1024], BF16)
    XS_d = dscr("XS_d", [NSLOT * 256, 1024], BF16)
    Y_d = dscr("Y_d", [NSLOT * 256, 1024], F32)
    WB_d = [nc.dram_tensor("WB%d_d" % i, [8192, 4096], BF16, kind="Internal").ap() for i in range(3)]
    AT_d = dscr("AT_d", [4096, 512], BF16) if debug else None
    RT_d = dscr("RT_d", [4096, 8], F32) if debug else None

    with ExitStack() as gs:
      try:
        fw = FW(nc, gs)
        gs_fw = [fw]

        def SB(st, name, shape, dt):
            return st.enter_context(nc.sbuf_tensor(name, list(shape), dt))

        def PS(st, name, shape, dt):
            return st.enter_context(nc.psum_tensor(name, list(shape), dt))

        def MM(o, lhsT, rhs, start=True, stop=True, r=(), w=(), sig=True):
            fw.op("pe", lambda e: e.matmul(o, lhsT=lhsT, rhs=rhs, start=start, stop=stop), r, w, sig)

        def TR(o, i, ident, r=(), w=(), sig=True):
            fw.op("pe", lambda e: e.transpose(out=o, in_=i, identity=ident), r, w, sig)

        def ACT(o, i, func, r=(), w=(), **kw):
            fw.op("act", lambda e: e.activation(out=o, in_=i, func=func, **kw), r, w)

        def CP(eng, o, i, r=(), w=()):
            if eng == "act":
                fw.op("act", lambda e: e.copy(out=o, in_=i), r, w)
            else:
                fw.op(eng, lambda e: e.tensor_copy(out=o, in_=i), r, w)

        def TT(eng, o, a, b, op, r=(), w=()):
            fw.op(eng, lambda e: e.tensor_tensor(out=o, in0=a, in1=b, op=op), r, w)

        def TS(eng, o, a, s1, s2, op0, op1=None, r=(), w=(), **kw):
            if op1 is None:
                fw.op(eng, lambda e: e.tensor_scalar(out=o, in0=a, scalar1=s1, scalar2=None, op0=op0, **kw), r, w)
            else:
                fw.op(eng, lambda e: e.tensor_scalar(out=o, in0=a, scalar1=s1, scalar2=s2, op0=op0, op1=op1, **kw), r, w)

        def STT(o, a, s, b, op0, op1, r=(), w=()):
            fw.op("dve", lambda e: e.scalar_tensor_tensor(out=o, in0=a, scalar=s, in1=b, op0=op0, op1=op1), r, w)

        def RSTD(src, bsrc, ss_, bss, div):
            fw.op("dve", lambda e: e.scalar_tensor_tensor(out=junk_g[:], in0=src, scalar=1.0, in1=src, op0=ALU.mult,
                                                          op1=ALU.mult, accum_out=ss_[:, 0:1]), [bsrc], [b_junk_g, bss])
            TS("dve", ss_[:, 1:2], ss_[:, 0:1], 1.0 / div, 1e-6, ALU.mult, ALU.add, r=[bss], w=[bss])
            TT("pool", ss_[:, 2:3], ss_[:, 1:2], misc[:, 81:82], ALU.pow, r=[bss, b_misc], w=[bss])

        def MEMSET(eng, o, v, w=()):
            fw.op(eng, lambda e: e.memset(o, v), (), w)

        def RED(o, i, op, r=(), w=()):
            fw.op("dve", lambda e: e.tensor_reduce(out=o, in_=i, axis=AX.X, op=op), r, w)

        def chk(k):
            if upto < k:
                fw.barrier()
                raise _Stop()

        ident_bf = SB(gs, "ident_bf", [128, 128], BF16); b_identbf = Buf()
        ident_f = SB(gs, "ident_f", [128, 128], F32); b_identf = Buf()
        tri_bf = SB(gs, "tri_bf", [128, 128], BF16); b_tri = Buf()
        tris_bf = SB(gs, "tris_bf", [128, 128], BF16); b_tris = Buf()
        ones_bf = SB(gs, "ones_bf", [128, 128], BF16); b_onesbf = Buf()
        ones_f = SB(gs, "ones_f", [128, 128], F32); b_onesf = Buf()
        misc = SB(gs, "misc", [128, 96], F32); b_misc = Buf()
        mod = SB(gs, "mod", [128, 6144], F32); b_mod = Buf()
        gfin = SB(gs, "gfin", [128, 1024], F32); b_gfin = Buf()
        junk_g = SB(gs, "junk_g", [128, 1024], BF16); b_junk_g = Buf()
        fw.dma("pool", ident_bf[:], c_ident, writes=[b_identbf])
        fw.dma("sp", ident_f[:], c_ident, writes=[b_identf])
        fw.dma("pool", tri_bf[:], c_tri, writes=[b_tri])
        fw.dma("pool", tris_bf[:], c_tris, writes=[b_tris])
        fw.dma("sp", misc[:], c_misc, writes=[b_misc])
        fw.dma("sp", gfin[:], final_g.partition_broadcast(128), writes=[b_gfin])
        MEMSET("dve", ones_bf[:], 1.0, w=[b_onesbf])
        MEMSET("dve", ones_f[:], 1.0, w=[b_onesf])
        SH1, G1, GT1, SH2, G2, GT2 = [mod[:, i * 1024:(i + 1) * 1024] for i in range(6)]

        chk(0)
        wst = ExitStack()
        win = SB(wst, "win", [128, 8, 4608], BF16); b_win = [Buf() for _ in range(8)]
        wba = SB(wst, "wba", [128, 4, 1024], BF16); b_wba = Buf()
        with ExitStack() as ph:
            adaw = [SB(ph, "adaw%d" % i, [128, 8, 1536], BF16) for i in range(2)]; b_adaw = [Buf(), Buf()]
            adab = SB(ph, "adab", [1, 6144], BF16); b_adab = Buf()
            c_sb = SB(ph, "c_sb", [128, 8], F32); b_csb = Buf()
            cT = SB(ph, "cT", [128, 8, 128], BF16); b_cT = Buf()
            ng = SB(ph, "ng", [128, 2048], F32); b_ng = Buf()
            pm = [PS(ph, "pm%d" % i, [128, 512], F32) for i in range(2)]; b_pm = [PBuf(), PBuf()]
            fw.dma("sp", c_sb[:], c_pk, writes=[b_csb])
            adv = ada_w.rearrange("(kc p) f -> p kc f", p=128)

            def load_ada(q):
                fw.dma("pool", adaw[q % 2][:], adv[:, :, q * 1536:(q + 1) * 1536], writes=[b_adaw[q % 2]])
            load_ada(0)
            load_ada(1)
            fw.dma("pool", adab[:], ada_b, writes=[b_adab])
            fw.dma("sp", ng[:, 0:1024], norm1_g.partition_broadcast(128), writes=[b_ng])
            fw.dma("sp", ng[:, 1024:2048], norm2_g.partition_broadcast(128), writes=[b_ng])
            for kc in range(8):
                CP("dve", cT[:, kc, :], c_sb[:, kc:kc + 1].to_broadcast([128, 128]), r=[b_csb], w=[b_cT])
            for n in range(12):
                q = n // 3
                p = pm[n % 2]; bp = b_pm[n % 2]
                c0 = (n % 3) * 512
                for kc in range(8):
                    MM(p[:, :], cT[:, kc, :], adaw[q % 2][:, kc, c0:c0 + 512], start=(kc == 0), stop=False,
                       r=[b_cT, b_adaw[q % 2]], w=[bp], sig=False)
                MM(p[:, :], ones_bf[0:1, :], adab[0:1, n * 512:(n + 1) * 512], start=False, stop=True,
                   r=[b_onesbf, b_adab], w=[bp])
                CP("act", mod[:, n * 512:(n + 1) * 512], p[:, :], r=[bp], w=[b_mod])
                if n % 3 == 2 and q + 2 < 4:
                    load_ada(q + 2)
                if n == 2:
                    for kc in range(8):
                        fw.dma("pool", win[:, kc, :], w_in[kc * 128:(kc + 1) * 128, :], writes=[b_win[kc]])
                    fw.dma("pool", wba[:], w_ba.rearrange("(kc p) f -> p kc f", p=128), writes=[b_wba])
            STT(G1, G1, 1.0, ng[:, 0:1024], ALU.add, ALU.mult, r=[b_mod, b_ng], w=[b_mod])
            STT(G2, G2, 1.0, ng[:, 1024:2048], ALU.add, ALU.mult, r=[b_mod, b_ng], w=[b_mod])
            fw.barrier()

        chk(1)
        with ExitStack() as ph:
            def D2(name, shape, dt):
                return [SB(ph, "%s%d" % (name, i), shape, dt) for i in range(2)], [Buf(), Buf()]
            wsp_f = SB(ph, "wsp_f", [128, 4, 128], F32); b_wspf = Buf()
            wspT = SB(ph, "wspT", [128, 4, 128], BF16); b_wspT = Buf()
            bsp = SB(ph, "bsp", [128, 4], F32); b_bsp = Buf()
            lng = SB(ph, "lng", [128, 512], F32); b_lng = Buf()
            lnb = SB(ph, "lnb", [128, 512], F32); b_lnb = Buf()
            pastb_sb = SB(ph, "pastb_sb", [128, 16, 32], F32); b_pastb = Buf()
            kmbd = SB(ph, "kmbd", [128, 4, 64], F32); b_kmbd = Buf()
            ksum0 = SB(ph, "ksum0", [128, 4], F32); b_ksum0 = Buf()
            xt, b_xt = D2("xt", [128, 1024], F32)
            rot, b_rot = D2("rot", [128, 128], F32)
            junk = SB(ph, "junk", [128, 1024], BF16); b_junk = Buf()
            ss = SB(ph, "ss", [128, 8], F32); b_ss = Buf()
            t32 = SB(ph, "t32", [128, 1024], F32); b_t32 = Buf()
            hb, b_hb = D2("hb", [128, 1024], BF16)
            hT, b_hT = D2("hT", [128, 8, 128], BF16)
            kr32, b_kr32 = D2("kr32", [128, 8, 64], F32)
            krb, b_krb = D2("krb", [128, 512], BF16)
            qr32, b_qr32 = D2("qr32", [128, 8, 64], F32)
            rtmp = [SB(ph, "rtmp%d" % i, [128, 8, 8], F32) for i in range(4)]; b_rtmp = [Buf() for _ in range(4)]
            vb = SB(ph, "vb", [128, 512], BF16); b_vb = Buf()
            kTs = SB(ph, "kTs", [128, 4, 128], BF16); b_kTs = Buf()
            qT32 = SB(ph, "qT32", [128, 4, 128], F32); b_qT32 = Buf()
            QA, b_QA = D2("QA", [128, 8, 96], BF16)
            gm = SB(ph, "gm", [128, 8, 32], F32); b_gm = Buf()
            top8 = SB(ph, "top8", [128, 8, 8], F32); b_top8 = Buf()
            thr = SB(ph, "thr", [128, 8], F32); b_thr = Buf()
            selm = SB(ph, "selm", [128, 8, 32], F32); b_selm = Buf()
            qtas = SB(ph, "qtas", [96, 8, 128], BF16); b_qtas = Buf()
            ug, b_ug = D2("ug", [128, 512], F32)
            vg = SB(ph, "vg", [128, 512], F32); b_vg = Buf()
            st6 = SB(ph, "st6", [128, 6], F32); b_st6 = Buf()
            mv = SB(ph, "mv", [128, 4], F32); b_mv = Buf()
            vn1 = SB(ph, "vn1", [128, 512], F32); b_vn1 = Buf()
            vnb, b_vnb = D2("vnb", [128, 512], BF16)
            zzb = SB(ph, "zzb", [128, 512], BF16); b_zzb = Buf()
            zTs = SB(ph, "zTs", [128, 4, 128], BF16); b_zTs = Buf()
            sga, b_sga = D2("sga", [128, 1024], F32)
            sgb = SB(ph, "sgb", [128, 1024], F32); b_sgb = Buf()
            GAt = SB(ph, "GAt", [128, 1024], F32); b_GAt = Buf()
            pT = PS(ph, "pT", [128, 1024], BF16); b_pT = PBuf()
            NPB = 3
            pP = [PS(ph, "pP%d" % i, [128, 512], F32) for i in range(NPB)]; b_pP = [PBuf() for _ in range(NPB)]
            pM = PS(ph, "pM", [128, 512], F32); b_pM = PBuf()
            pT2 = PS(ph, "pT2", [128, 1024], BF16); b_pT2 = PBuf()
            pQ = PS(ph, "pQ", [128, 512], F32); b_pQ = PBuf()
            pQA = PS(ph, "pQA", [128, 1024], BF16); b_pQA = PBuf()
            pSV = pQ; b_pSV = b_pQ

            fw.dma("sp", wsp_f[:], w_sp.rearrange("g t s -> t g s"), writes=[b_wspf])
            fw.dma("sp", bsp[:], b_spT, writes=[b_bsp])
            fw.dma("sp", lng[:], ln_g.partition_broadcast(128), writes=[b_lng])
            fw.dma("sp", lnb[:], ln_b.partition_broadcast(128), writes=[b_lnb])
            fw.dma("sp", pastb_sb[:].rearrange("p a b -> p (a b)"), pastb.partition_broadcast(128), writes=[b_pastb])
            MEMSET("dve", kmbd[:], 0.0, w=[b_kmbd])
            for g in range(4):
                TR(pQ[:, g * 128:(g + 1) * 128], wsp_f[:, g, :], ident_f[:], r=[b_wspf, b_identf], w=[b_pQ], sig=(g == 3))
            TT("dve", wspT[:], pQ[:, :].rearrange("p (g t) -> p g t", g=4),
               tri_bf[:].unsqueeze(1).to_broadcast([128, 4, 128]), ALU.mult, r=[b_pQ, b_tri], w=[b_wspT])

            pcnt = [0]

            hpar = [0]

            def proj(c0):
                i = pcnt[0] % NPB
                pcnt[0] += 1
                hj = hpar[0]
                for kc in range(8):
                    MM(pP[i][:, :], hT[hj][:, kc, :], win[:, kc, c0:c0 + 512], start=(kc == 0), stop=(kc == 7),
                       r=[b_hT[hj], b_win[kc]], w=[b_pP[i]], sig=(kc == 7))
                return pP[i], b_pP[i]

            def HTR(n):
                j = n % 2
                for kc in range(8):
                    TR(pT[:, kc * 128:(kc + 1) * 128], hb[j][:, kc * 128:(kc + 1) * 128], ident_bf[:],
                       r=[b_hb[j], b_identbf], w=[b_pT], sig=(kc == 7))
                CP("act", hT[j][:].rearrange("p a b -> p (a b)"), pT[:, :], r=[b_pT], w=[b_hT[j]])

            def rotary(P, bP, dst, b_dst, rt, b_rt):
                Pv = P[:, :].rearrange("p (h d) -> p h d", h=8)
                cos = rt[:, 0:64].rearrange("p (h d) -> p h d", h=8)
                sin = rt[:, 64:128].rearrange("p (h d) -> p h d", h=8)
                CP("act", dst[:], Pv, r=[bP], w=[b_dst])
                TT("dve", rtmp[0][:], Pv[:, :, 0:8], cos, ALU.mult, r=[bP, b_rt], w=[b_rtmp[0]])
                TT("dve", rtmp[1][:], Pv[:, :, 8:16], sin, ALU.mult, r=[bP, b_rt], w=[b_rtmp[1]])
                TT("dve", rtmp[2][:], Pv[:, :, 8:16], cos, ALU.mult, r=[bP, b_rt], w=[b_rtmp[2]])
                TT("dve", rtmp[3][:], Pv[:, :, 0:8], sin, ALU.mult, r=[bP, b_rt], w=[b_rtmp[3]])
                TT("dve", dst[:, :, 0:8], rtmp[0][:], rtmp[1][:], ALU.subtract, r=[b_rtmp[0], b_rtmp[1]], w=[b_dst])
                TT("dve", dst[:, :, 8:16], rtmp[2][:], rtmp[3][:], ALU.add, r=[b_rtmp[2], b_rtmp[3]], w=[b_dst])

            tiles = []
            for i in range(NPAIR):
                for own in (False, True):
                    for tt in range(2):
                        tiles.append((i, own, tt))
            NT = len(tiles)

            def NORM(n):
                i, own, tt = tiles[n]
                j = n % 2
                row = (2 * i + tt) * 128
                xs_, rs_ = (x_own, rot_own) if own else (x_oth, rot_oth)
                fw.dma("pool", xt[j][:], xs_[row:row + 128, :], writes=[b_xt[j]])
                fw.dma("pool", rot[j][:], rs_[row:row + 128, :], writes=[b_rot[j]])
                RSTD(xt[j][:], b_xt[j], ss, b_ss, 1024)
                STT(t32[:], xt[j][:], ss[:, 2:3], G1, ALU.mult, ALU.mult, r=[b_xt[j], b_ss, b_mod], w=[b_t32])
                TT("pool", hb[j][:], t32[:], SH1, ALU.add, r=[b_t32, b_mod], w=[b_hb[j]])

            def HEAD(n, fills=()):
                fills = list(fills)

                def fill():
                    if fills:
                        fills.pop(0)()

                def fill_rest():
                    while fills:
                        fills.pop(0)()
                i, own, tt = tiles[n]
                j = n % 2
                kti = 4 * i + (2 if own else 0) + tt
                ot = 2 * i + tt
                if n == 0:
                    HTR(0)
                hpar[0] = j
                P, bP = proj(1536)
                rotary(P, bP, kr32[j], b_kr32[j], rot[j], b_rot[j])
                CP("pool", krb[j][:], kr32[j][:].rearrange("p h d -> p (h d)"), r=[b_kr32[j]], w=[b_krb[j]])
                fill()
                P, bP = proj(2048)
                CP("act", vb[:], P[:, :], r=[bP], w=[b_vb])
                fw.dma("sp", V_d[kti * 128:(kti + 1) * 128, :], vb[:], reads=[b_vb])
                fill()
                if not own:
                    if n + 1 < NT:
                        HTR(n + 1)
                    fill_rest()
                    return
                P, bP = proj(1024)
                rotary(P, bP, qr32[j], b_qr32[j], rot[j], b_rot[j])
                ACT(QA[j][:, :, 0:64], qr32[j][:], AF.Copy, r=[b_qr32[j]], w=[b_QA[j]], scale=0.125)
                fill()
                P, bP = proj(0)
                ACT(ug[j][:], P[:, :], AF.Gelu_apprx_tanh, r=[bP], w=[b_ug[j]])
                fill()
                P, bP = proj(512)
                ACT(vg[:], P[:, :], AF.Gelu_apprx_tanh, r=[bP], w=[b_vg])
                fill()
                for g in range(2):
                    P, bP = proj(2560 + g * 512)
                    ACT(sga[j][:, g * 512:(g + 1) * 512], P[:, :], AF.Sigmoid, r=[bP], w=[b_sga[j]])
                    if g == 0:
                        fw.op("dve", lambda e: e.bn_stats(out=st6[:], in_=vg[:]), [b_vg], [b_st6])
                        fw.op("dve", lambda e: e.bn_aggr(out=mv[:, 0:2], in_=st6[:]), [b_st6], [b_mv])
                        TS("dve", mv[:, 2:3], mv[:, 1:2], 1e-6, None, ALU.add, r=[b_mv], w=[b_mv])
                        TT("pool", mv[:, 3:4], mv[:, 2:3], misc[:, 81:82], ALU.pow, r=[b_mv, b_misc], w=[b_mv])
                        TS("dve", vn1[:], vg[:], mv[:, 0:1], mv[:, 3:4], ALU.subtract, ALU.mult, r=[b_vg, b_mv], w=[b_vn1])
                        TT("pool", vn1[:], vn1[:], lng[:], ALU.mult, r=[b_vn1, b_lng], w=[b_vn1])
                        TT("dve", vnb[j][:], vn1[:], lnb[:], ALU.add, r=[b_vn1, b_lnb], w=[b_vnb[j]])
                if n + 1 < NT:
                    HTR(n + 1)
                for g in range(2):
                    P, bP = proj(3584 + g * 512)
                    ACT(sgb[:, g * 512:(g + 1) * 512], P[:, :], AF.Sigmoid, r=[bP], w=[b_sgb])
                fw.dma("sp", GB_d[ot * 128:(ot + 1) * 128, :], sgb[:], reads=[b_sgb])
                fill_rest()

            def TAIL(n):
                i, own, tt = tiles[n]
                j = n % 2
                kti = 4 * i + (2 if own else 0) + tt
                pb = 2 * i + (1 if own else 0)
                ot = 2 * i + tt
                def p1():
                    for hp in range(4):
                        TR(pT2[:, hp * 128:(hp + 1) * 128], krb[j][:, hp * 128:(hp + 1) * 128], ident_bf[:],
                           r=[b_krb[j], b_identbf], w=[b_pT2], sig=(hp == 3))
                    CP("act", kTs[:].rearrange("p a b -> p (a b)"), pT2[:, 0:512], r=[b_pT2], w=[b_kTs])
                    fw.dma("sp", KT_d[:, :, kti * 128:(kti + 1) * 128].rearrange("hp r t -> r hp t"), kTs[:], reads=[b_kTs])
                    kr32f = kr32[j][:].rearrange("p h d -> p (h d)")
                    for hp in range(4):
                        MM(pM[:, hp:hp + 1], kr32f[:, hp * 128:(hp + 1) * 128], ones_f[:, 0:1],
                           r=[b_kr32[j], b_onesf], w=[b_pM], sig=(hp == 3))
                    if tt == 0:
                        ACT(ksum0[:], pM[:, 0:4], AF.Copy, r=[b_pM], w=[b_ksum0], scale=1.0 / 256)
                    else:
                        STT(kmbd[0:64, :, pb], pM[0:64, 0:4], 1.0 / 256, ksum0[0:64, :], ALU.mult, ALU.add,
                            r=[b_pM, b_ksum0], w=[b_kmbd])
                        STT(kmbd[64:128, :, 32 + pb], pM[64:128, 0:4], 1.0 / 256, ksum0[64:128, :], ALU.mult, ALU.add,
                            r=[b_pM, b_ksum0], w=[b_kmbd])
                    if not own:
                        return
                    qr32f = qr32[j][:].rearrange("p h d -> p (h d)")
                    for hp in range(4):
                        TR(pQ[:, hp * 128:(hp + 1) * 128], qr32f[:, hp * 128:(hp + 1) * 128], ident_f[:],
                           r=[b_qr32[j], b_identf], w=[b_pQ], sig=(hp == 3))
                    CP("act", qT32[:].rearrange("p a b -> p (a b)"), pQ[:, :], r=[b_pQ], w=[b_qT32])
                    for g in range(4):
                        MM(pSV[:, g * 128:(g + 1) * 128], wspT[:, g, :], vnb[j][:, g * 128:(g + 1) * 128],
                           r=[b_wspT, b_vnb[j]], w=[b_pSV], sig=(g == 3))
                def p2():
                    for hp in range(4):
                        MM(pM[:, 256 + hp * 64:256 + (hp + 1) * 64], qT32[:, hp, :], kmbd[:, hp, :],
                           r=[b_qT32, b_kmbd], w=[b_pM], sig=(hp == 3))
                    for g in range(4):
                        STT(zzb[:, g * 128:(g + 1) * 128], pSV[:, g * 128:(g + 1) * 128], bsp[:, g:g + 1],
                            ug[j][:, g * 128:(g + 1) * 128], ALU.add, ALU.mult, r=[b_pSV, b_bsp, b_ug[j]], w=[b_zzb])
                def p3():
                    TT("dve", gm[:], pM[:, 256:512].rearrange("p (h j) -> p h j", h=8),
                       pastb_sb[:, i, :].unsqueeze(1).to_broadcast([128, 8, 32]), ALU.add, r=[b_pM, b_pastb], w=[b_gm])
                    for h in range(8):
                        fw.op("dve", lambda e, h=h: e.max(out=top8[:, h, :], in_=gm[:, h, :]), [b_gm], [b_top8])
                    TS("dve", thr[:], top8[:, :, 2], -1e29, None, ALU.max, r=[b_top8], w=[b_thr])
                    TT("dve", selm[:], gm[:], thr[:].unsqueeze(2).to_broadcast([128, 8, 32]), ALU.is_ge, r=[b_gm, b_thr], w=[b_selm])
                    TS("dve", QA[j][:, :, 64:96], selm[:], -1.0, -NEG, ALU.add, ALU.mult, r=[b_selm], w=[b_QA[j]])
                    MEMSET("dve", QA[j][:, :, 64 + pb:65 + pb], 0.0, w=[b_QA[j]])
                    for kc in range(4):
                        TR(pT2[:, 512 + kc * 128:512 + (kc + 1) * 128], zzb[:, kc * 128:(kc + 1) * 128], ident_bf[:],
                           r=[b_zzb, b_identbf], w=[b_pT2], sig=(kc == 3))
                    CP("act", zTs[:].rearrange("p a b -> p (a b)"), pT2[:, 512:1024], r=[b_pT2], w=[b_zTs])
                def p4():
                    for g in range(2):
                        jj = pcnt[0] % NPB
                        pcnt[0] += 1
                        for kc in range(4):
                            MM(pP[jj][:, :], zTs[:, kc, :], wba[:, kc, g * 512:(g + 1) * 512], start=(kc == 0), stop=(kc == 3),
                               r=[b_zTs, b_wba], w=[b_pP[jj]], sig=(kc == 3))
                        TT("dve", GAt[:, g * 512:(g + 1) * 512], pP[jj][:, :], sga[j][:, g * 512:(g + 1) * 512], ALU.mult,
                           r=[b_pP[jj], b_sga[j]], w=[b_GAt])
                    fw.dma("sp", GA_d[ot * 128:(ot + 1) * 128, :], GAt[:], reads=[b_GAt])
                def p5():
                    for h in range(8):
                        TR(pQA[0:96, h * 128:(h + 1) * 128], QA[j][:, h, :], ident_bf[:], r=[b_QA[j], b_identbf], w=[b_pQA], sig=(h == 7))
                    CP("act", qtas[:].rearrange("p a b -> p (a b)"), pQA[0:96, :], r=[b_pQA], w=[b_qtas])
                    fw.dma("sp", QT_d[:, :, ot * 128:(ot + 1) * 128].rearrange("h r t -> r h t"), qtas[:], reads=[b_qtas])
                return [p1, p2, p3, p4, p5] if own else [p1]

            NORM(0)
            NORM(1)
            HEAD(0)
            for n in range(NT):
                if n + 2 < NT:
                    NORM(n + 2)
                pieces = TAIL(n)
                if n + 1 < NT:
                    HEAD(n + 1, pieces)
                else:
                    for p_ in pieces:
                        p_()
            fw.barrier()

        wst.close()
        wbb = SB(gs, "wbb", [128, 4, 1024], BF16); b_wbb = Buf()
        wo = SB(gs, "wo", [128, 8, 1024], BF16); b_wo = Buf()
        wr = SB(gs, "wr", [128, 8, 36], F32); b_wr = Buf()
        br = SB(gs, "br", [1, 36], F32); b_br = Buf()
        fw.dma("pool", wbb[:], w_bb.rearrange("(kc p) f -> p kc f", p=128), writes=[b_wbb])
        fw.dma("pool", wo[:], w_out.rearrange("(kc p) f -> p kc f", p=128), writes=[b_wo])
        fw.dma("sp", wr[:], w_rt.rearrange("(kc p) f -> p kc f", p=128), writes=[b_wr])
        fw.dma("sp", br[:], b_rt, writes=[b_br])
        attn = SB(gs, "attn", [128, NOT, 512], BF16); b_attn = [Buf() for _ in range(NOT)]

        chk(2)
        with ExitStack() as ph:
            kta = [SB(ph, "kta%d" % i, [96, 8192], BF16) for i in range(2)]; b_kta = [Buf(), Buf()]
            vsb = [SB(ph, "vsb%d" % i, [128, 64, 65], BF16) for i in range(2)]; b_vsb = [Buf(), Buf()]
            qta = [SB(ph, "qta%d" % i, [96, 4096], BF16) for i in range(2)]; b_qta = [Buf(), Buf()]
            pTt = [SB(ph, "pTt%d" % i, [128, 512], BF16) for i in range(4)]; b_pTt = [Buf() for _ in range(4)]
            rden = SB(ph, "rden", [128, 4], F32); b_rden = Buf()
            pS = [PS(ph, "pS%d" % i, [128, 512], F32) for i in range(4)]; b_pS = [PBuf() for _ in range(4)]
            pO = [PS(ph, "pO%d" % i, [128, 512], F32) for i in range(4)]; b_pO = [PBuf() for _ in range(4)]
            cv = [SB(ph, "cv%d" % i, [128, 4096], BF16) for i in range(4)]; b_cv = [Buf() for _ in range(4)]
            w_src = [w_gate.rearrange("e (p k) f -> e p (k f)", k=8), w_up.rearrange("e (p k) f -> e p (k f)", k=8),
                     w_down.rearrange("e (p k) f -> e p (k f)", k=4)]

            def conv(u):
                e_, m_ = u // 3, u % 3
                c_ = u % 4
                fw.dma("pool", cv[c_][:], w_src[m_][e_], writes=[b_cv[c_]])
                fw.dma("sp", WB_d[m_][e_ * 128:(e_ + 1) * 128, :], cv[c_][:], reads=[b_cv[c_]])

            for bfi in range(2):
                fw.dma("pool", kta[bfi][64:96, :], c_onehot, writes=[b_kta[bfi]])
                MEMSET("pool", vsb[bfi][:, :, 64:65], 1.0, w=[b_vsb[bfi]])

            def load_head(h):
                bfi = h % 2
                fw.dma("sp", kta[bfi][0:64, :], KT_d[h // 2, (h % 2) * 64:(h % 2) * 64 + 64, :], writes=[b_kta[bfi]])
                vv = V_d[:, h * 64:(h + 1) * 64].rearrange("(t p) d -> p t d", p=128)
                for t8_ in range(8):
                    fw.dma("sp", vsb[bfi][:, t8_ * 8:(t8_ + 1) * 8, 0:64], vv[:, t8_ * 8:(t8_ + 1) * 8, :], writes=[b_vsb[bfi]])
                fw.dma("sp", qta[bfi][:], QT_d[h], writes=[b_qta[bfi]])

            items = []

            def mk_gov(h, i, pr, s, O, bO, first0):
                bfi = h % 2
                K = kta[bfi]; V = vsb[bfi]; qs = qta[bfi][:, i * 256:(i + 1) * 256]
                rds = [b_kta[bfi], b_qta[bfi]]

                def qk():
                    for a in range(2):
                        kt = 2 * pr + a
                        MM(pS[s][:, a * 256:(a + 1) * 256], K[:, kt * 128:(kt + 1) * 128], qs, r=rds, w=[b_pS[s]], sig=(a == 1))

                def post():
                    ACT(pTt[s][:], pS[s][:, :], AF.Exp, r=[b_pS[s]], w=[b_pTt[s]])

                def pv():
                    for a in range(2):
                        kt = 2 * pr + a
                        for q in range(2):
                            MM(O[q][:, 0:65], pTt[s][:, a * 256 + q * 128:a * 256 + (q + 1) * 128], V[:, kt, :],
                               start=(first0 and a == 0), stop=False, r=[b_pTt[s], b_vsb[bfi]], w=[bO[q]], sig=False)
                return qk, post, pv

            def mk_diag(h, i, s, O, bO):
                bfi = h % 2
                K = kta[bfi]; V = vsb[bfi]; qs = qta[bfi][:, i * 256:(i + 1) * 256]
                rds = [b_kta[bfi], b_qta[bfi]]
                kt0 = 4 * i + 2

                def qk():
                    MM(pS[s][:, 0:256], K[:, kt0 * 128:(kt0 + 1) * 128], qs, r=rds, w=[b_pS[s]], sig=False)
                    MM(pS[s][:, 256:384], K[:, (kt0 + 1) * 128:(kt0 + 2) * 128], qs[:, 128:256], r=rds, w=[b_pS[s]])

                def post():
                    ACT(pTt[s][:, 0:384], pS[s][:, 0:384], AF.Exp, r=[b_pS[s]], w=[b_pTt[s]])
                    TT("dve", pTt[s][:, 0:128], pTt[s][:, 0:128], tri_bf[:], ALU.mult, r=[b_pTt[s], b_tri], w=[b_pTt[s]])
                    TT("pool", pTt[s][:, 256:384], pTt[s][:, 256:384], tri_bf[:], ALU.mult, r=[b_pTt[s], b_tri], w=[b_pTt[s]])

                def pv():
                    MM(O[0][:, 0:65], pTt[s][:, 0:128], V[:, kt0, :], start=False, stop=True,
                       r=[b_pTt[s], b_vsb[bfi]], w=[bO[0]])
                    MM(O[1][:, 0:65], pTt[s][:, 128:256], V[:, kt0, :], start=False, stop=False,
                       r=[b_pTt[s], b_vsb[bfi]], w=[bO[1]], sig=False)
                    MM(O[1][:, 0:65], pTt[s][:, 256:384], V[:, kt0 + 1, :], start=False, stop=True,
                       r=[b_pTt[s], b_vsb[bfi]], w=[bO[1]])
                    for q in range(2):
                        ot = 2 * i + q
                        fw.op("dve", lambda e, q=q: e.reciprocal(out=rden[:, q:q + 1], in_=O[q][:, 64:65]), [bO[q]], [b_rden])
                        TS("dve", attn[:, ot, h * 64:(h + 1) * 64], O[q][:, 0:64], rden[:, q:q + 1], None, ALU.mult,
                           r=[bO[q], b_rden], w=[b_attn[ot]])
                return qk, post, pv

            sc = 0
            oc = 0
            for h in range(8):
                for i in range(NPAIR):
                    O = [pO[(oc % 2) * 2 + q] for q in range(2)]
                    bO = [b_pO[(oc % 2) * 2 + q] for q in range(2)]
                    oc += 1
                    for pr in range(2 * i + 1):
                        it = mk_gov(h, i, pr, sc % 4, O, bO, pr == 0)
                        items.append((it, h if (i == 0 and pr == 0) else None))
                        sc += 1
                    items.append((mk_diag(h, i, sc % 4, O, bO), None))
                    sc += 1
            load_head(0)
            LA = 3
            for n in range(len(items) + LA):
                if n % 22 == 0 and n // 22 < 96:
                    conv(n // 22)
                if n < len(items):
                    items[n][0][0]()
                if n - LA >= 0:
                    (qk, post, pv), hstart = items[n - LA]
                    post()
                    pv()
                    if hstart is not None and hstart + 1 < 8:
                        load_head(hstart + 1)
            if debug:
                for ot in range(NOT):
                    fw.dma("sp", AT_d[ot * 128:(ot + 1) * 128, :], attn[:, ot, :], reads=[b_attn[ot]])
            fw.barrier()

        A_all = SB(gs, "A_all", [128, NOT, 32], F32); b_A = Buf()
        W_all = SB(gs, "W_all", [128, NOT, 32], F32); b_W = Buf()
        R_all = SB(gs, "R_all", [128, NOT, 32], F32); b_R = Buf()
        base = SB(gs, "base", [128, 32], F32); b_base = Buf()
        idx_all = SB(gs, "idx_all", [128, NOT, 2], I32); b_idx = Buf()
        wsel = SB(gs, "wsel", [128, NOT, 2], F32); b_wsel = Buf()
        widx = SB(gs, "widx", [128, NSLOT], I32); b_widx = Buf()
        b_XS = Buf(); b_Y = Buf()

        chk(3)
        with ExitStack() as ph:
            def D2(name, shape, dt):
                return [SB(ph, "%s%d" % (name, i), shape, dt) for i in range(2)], [Buf(), Buf()]
            gat, b_gat = D2("gat", [128, 1024], F32)
            gbt, b_gbt = D2("gbt", [128, 1024], F32)
            xt, b_xt = D2("xtc", [128, 1024], F32)
            aT = SB(ph, "aT", [128, 4, 128], BF16); b_aT = Buf()
            t32a = SB(ph, "t32a", [128, 1024], F32); b_t32a = Buf()
            t32b = SB(ph, "t32b", [128, 1024], F32); b_t32b = Buf()
            t32c = SB(ph, "t32c", [128, 1024], F32); b_t32c = Buf()
            mb, b_mb = D2("mb", [128, 1024], BF16)
            mT = SB(ph, "mT", [128, 8, 128], BF16); b_mT = Buf()
            x1, b_x1 = D2("x1", [128, 1024], F32)
            h2, b_h2 = D2("h2", [128, 1024], F32)
            h2b = SB(ph, "h2b", [128, 1024], BF16); b_h2b = Buf()
            h2T = SB(ph, "h2T", [128, 8, 128], F32); b_h2T = Buf()
            junk = SB(ph, "junkc", [128, 1024], BF16); b_junk = Buf()
            ss = SB(ph, "ssc", [128, 8], F32); b_ss = Buf()
            lgall = SB(ph, "lgall", [128, NOT, 36], F32); b_lgall = Buf()
            gmax = SB(ph, "gmax", [128, NOT], F32); b_gmax = Buf()
            gsum = SB(ph, "gsum", [128, NOT], F32); b_gsum = Buf()
            m1 = SB(ph, "m1", [128, NOT], F32); b_m1 = Buf()
            m2 = SB(ph, "m2", [128, NOT], F32); b_m2 = Buf()
            ohg = SB(ph, "ohg", [128, NOT, 4], F32); b_ohg = Buf()
            gtmp = SB(ph, "gtmp", [128, NOT, 4], F32); b_gtmp = Buf()
            em = SB(ph, "em", [128, NOT, 32], F32); b_em = Buf()
            eq1 = SB(ph, "eq1", [128, NOT, 32], F32); b_eq1 = Buf()
            csum = SB(ph, "csum", [128, NOT, 32], F32); b_csum = Buf()
            abf = SB(ph, "abf", [128, NOT, 32], BF16); b_abf = Buf()
            pTa = PS(ph, "pTa", [128, 1024], BF16); b_pTa = PBuf()
            pTm = PS(ph, "pTm", [128, 1024], BF16); b_pTm = PBuf()
            pP = [PS(ph, "pPc%d" % i, [128, 512], F32) for i in range(2)]; b_pP = [PBuf(), PBuf()]
            pH = [PS(ph, "pH%d" % i, [128, 512], F32) for i in range(2)]; b_pH = [PBuf(), PBuf()]
            pL = PS(ph, "pL", [128, 512], F32); b_pL = PBuf()

            MEMSET("dve", base[:], 0.0, w=[b_base])
            zt = SB(ph, "zt", [128, 4, 1024], BF16); b_zt = Buf()
            MEMSET("pool", zt[:], 0.0, w=[b_zt])
            for s_ in range(NSLOT // 2):
                fw.dma("sp", XS_d[s_ * 512:(s_ + 1) * 512, :].rearrange("(r p) f -> p r f", p=128), zt[:], reads=[b_zt])

            pc = [0]

            def load_g(ot):
                j = ot % 2
                fw.dma("act", gat[j][:], GA_d[ot * 128:(ot + 1) * 128, :], writes=[b_gat[j]])
                fw.dma("act", gbt[j][:], GB_d[ot * 128:(ot + 1) * 128, :], writes=[b_gbt[j]])

            def S1a(ot):
                j = ot % 2
                fw.dma("act", xt[j][:], x_own[ot * 128:(ot + 1) * 128, :], writes=[b_xt[j]])
                for kc in range(4):
                    TR(pTa[:, kc * 128:(kc + 1) * 128], attn[:, ot, kc * 128:(kc + 1) * 128], ident_bf[:],
                       r=[b_attn[ot], b_identbf], w=[b_pTa], sig=(kc == 3))
                CP("act", aT[:].rearrange("p a b -> p (a b)"), pTa[:, 0:512], r=[b_pTa], w=[b_aT])

            def S1b(ot):
                j = ot % 2
                for g in range(2):
                    k_ = pc[0] % 2; pc[0] += 1
                    for kc in range(4):
                        MM(pP[k_][:, :], aT[:, kc, :], wbb[:, kc, g * 512:(g + 1) * 512], start=(kc == 0), stop=(kc == 3),
                           r=[b_aT, b_wbb], w=[b_pP[k_]], sig=(kc == 3))
                    TT("dve", t32a[:, g * 512:(g + 1) * 512], pP[k_][:, :], gbt[j][:, g * 512:(g + 1) * 512], ALU.mult,
                       r=[b_pP[k_], b_gbt[j]], w=[b_t32a])
                TT("dve", mb[j][:], t32a[:], gat[j][:], ALU.add, r=[b_t32a, b_gat[j]], w=[b_mb[j]])

            def S2a(ot):
                j = ot % 2
                for kc in range(8):
                    TR(pTm[:, kc * 128:(kc + 1) * 128], mb[j][:, kc * 128:(kc + 1) * 128], ident_bf[:],
                       r=[b_mb[j], b_identbf], w=[b_pTm], sig=(kc == 7))
                CP("act", mT[:].rearrange("p a b -> p (a b)"), pTm[:, :], r=[b_pTm], w=[b_mT])

            def S2b(ot):
                j = ot % 2
                for g in range(2):
                    k_ = pc[0] % 2; pc[0] += 1
                    for kc in range(8):
                        MM(pP[k_][:, :], mT[:, kc, :], wo[:, kc, g * 512:(g + 1) * 512], start=(kc == 0), stop=(kc == 7),
                           r=[b_mT, b_wo], w=[b_pP[k_]], sig=(kc == 7))
                    TT("dve", t32b[:, g * 512:(g + 1) * 512], pP[k_][:, :], GT1[:, g * 512:(g + 1) * 512], ALU.mult,
                       r=[b_pP[k_], b_mod], w=[b_t32b])
                TT("pool", x1[j][:], t32b[:], xt[j][:], ALU.add, r=[b_t32b, b_xt[j]], w=[b_x1[j]])
                fw.dma("sp", X1_d[ot * 128:(ot + 1) * 128, :], x1[j][:], reads=[b_x1[j]])

            def S3(ot):
                j = ot % 2
                RSTD(x1[j][:], b_x1[j], ss, b_ss, 1024)
                STT(t32c[:], x1[j][:], ss[:, 2:3], G2, ALU.mult, ALU.mult, r=[b_x1[j], b_ss, b_mod], w=[b_t32c])
                TT("pool", h2[j][:], t32c[:], SH2, ALU.add, r=[b_t32c, b_mod], w=[b_h2[j]])
                CP("act", h2b[:], h2[j][:], r=[b_h2[j]], w=[b_h2b])
                fw.dma("sp", H2_d[ot * 128:(ot + 1) * 128, :], h2b[:], reads=[b_h2b])

            def S4a(ot):
                j = ot % 2
                for kc in range(8):
                    TR(pH[kc // 4][:, (kc % 4) * 128:(kc % 4 + 1) * 128], h2[j][:, kc * 128:(kc + 1) * 128], ident_f[:],
                       r=[b_h2[j], b_identf], w=[b_pH[kc // 4]], sig=(kc % 4 == 3))
                for hh in range(2):
                    CP("act" if hh == 0 else "dve", h2T[:, hh * 4:(hh + 1) * 4, :].rearrange("p a b -> p (a b)"), pH[hh][:, :],
                       r=[b_pH[hh]], w=[b_h2T])

            def S4b(ot):
                j = ot % 2
                for kc in range(8):
                    MM(pL[:, 0:36], h2T[:, kc, :], wr[:, kc, :], start=(kc == 0), stop=False, r=[b_h2T, b_wr], w=[b_pL], sig=False)
                MM(pL[:, 0:36], ones_f[0:1, :], br[0:1, :], start=False, stop=True, r=[b_onesf, b_br], w=[b_pL])
                CP("act", lgall[:, ot, :], pL[:, 0:36], r=[b_pL], w=[b_lgall])

            load_g(0)
            for k in range(NOT + 3):
                def ok(t):
                    return 0 <= t < NOT
                if ok(k + 1):
                    load_g(k + 1)
                if ok(k): S1a(k)
                if ok(k - 1): S2a(k - 1)
                if ok(k - 3): S4a(k - 3)
                if ok(k): S1b(k)
                if ok(k - 1): S2b(k - 1)
                if ok(k - 2): S3(k - 2)
                if ok(k - 3): S4b(k - 3)
            T = NOT
            G = lgall[:, :, 0:4]
            E4 = lgall[:, :, 4:36].rearrange("p t (g e) -> p t g e", g=4)
            bc3 = lambda a, n: a.unsqueeze(2).to_broadcast([128, T, n])
            RED(gmax[:], G, ALU.max, r=[b_lgall], w=[b_gmax])
            TT("dve", ohg[:], G, bc3(gmax[:], 4), ALU.is_equal, r=[b_lgall, b_gmax], w=[b_ohg])
            TT("dve", gtmp[:], G, bc3(gmax[:], 4), ALU.subtract, r=[b_lgall, b_gmax], w=[b_gtmp])
            ACT(gtmp[:], gtmp[:], AF.Exp, r=[b_gtmp], w=[b_gtmp])
            RED(gsum[:], gtmp[:], ALU.add, r=[b_gtmp], w=[b_gsum])
            fw.op("dve", lambda e: e.reciprocal(out=gsum[:], in_=gsum[:]), [b_gsum], [b_gsum])
            TS("dve", ohg[:], ohg[:], -1.0, 1e30, ALU.add, ALU.mult, r=[b_ohg], w=[b_ohg])
            TT("dve", em[:].rearrange("p t (g e) -> p t g e", g=4), E4,
               ohg[:].unsqueeze(3).to_broadcast([128, T, 4, 8]), ALU.add, r=[b_lgall, b_ohg], w=[b_em])
            RED(m1[:], em[:], ALU.max, r=[b_em], w=[b_m1])
            TT("dve", eq1[:], em[:], bc3(m1[:], 32), ALU.is_equal, r=[b_em, b_m1], w=[b_eq1])
            STT(eq1[:].rearrange("p a b -> p (a b)"), eq1[:].rearrange("p a b -> p (a b)"), -1e30,
                em[:].rearrange("p a b -> p (a b)"), ALU.mult, ALU.add, r=[b_eq1, b_em], w=[b_eq1])
            RED(m2[:], eq1[:], ALU.max, r=[b_eq1], w=[b_m2])
            TT("dve", A_all[:], em[:], bc3(m2[:], 32), ALU.is_ge, r=[b_em, b_m2], w=[b_A])
            TT("dve", eq1[:], em[:], bc3(m1[:], 32), ALU.subtract, r=[b_em, b_m1], w=[b_eq1])
            ACT(eq1[:], eq1[:], AF.Exp, r=[b_eq1], w=[b_eq1])
            TT("dve", eq1[:], eq1[:], A_all[:], ALU.mult, r=[b_eq1, b_A], w=[b_eq1])
            RED(m1[:], eq1[:], ALU.add, r=[b_eq1], w=[b_m1])
            fw.op("dve", lambda e: e.reciprocal(out=m1[:], in_=m1[:]), [b_m1], [b_m1])
            TT("dve", m1[:], m1[:], gsum[:], ALU.mult, r=[b_m1, b_gsum], w=[b_m1])
            TT("dve", W_all[:], eq1[:], bc3(m1[:], 32), ALU.mult, r=[b_eq1, b_m1], w=[b_W])
            CP("dve", abf[:], A_all[:], r=[b_A], w=[b_abf])
            abf2 = abf[:].rearrange("p a b -> p (a b)")
            for hh in range(2):
                MM(pH[hh][:, :], tris_bf[:], abf2[:, hh * 512:(hh + 1) * 512], r=[b_tris, b_abf], w=[b_pH[hh]])
                MM(pP[hh][:, :], ones_bf[:], abf2[:, hh * 512:(hh + 1) * 512], r=[b_onesbf, b_abf], w=[b_pP[hh]])
            cs = [eq1, em]; b_cs = [b_eq1, b_em]
            for hh in range(2):
                CP("act", cs[0][:, hh * 16:(hh + 1) * 16, :].rearrange("p a b -> p (a b)"), pP[hh][:, :], r=[b_pP[hh]], w=[b_cs[0]])
            CP("dve", csum[:], cs[0][:], r=[b_cs[0]], w=[b_csum])
            cur = 0
            for sh in (1, 2, 4, 8, 16):
                nx = 1 - cur
                CP("dve", cs[nx][:, 0:sh, :], cs[cur][:, 0:sh, :], r=[b_cs[cur]], w=[b_cs[nx]])
                TT("dve", cs[nx][:, sh:T, :], cs[cur][:, sh:T, :], cs[cur][:, 0:T - sh, :], ALU.add, r=[b_cs[cur]], w=[b_cs[nx]])
                cur = nx
            CP("dve", base[:], cs[cur][:, T - 1, :], r=[b_cs[cur]], w=[b_base])
            TT("dve", csum[:], cs[cur][:], csum[:], ALU.subtract, r=[b_cs[cur], b_csum], w=[b_csum])
            for hh in range(2):
                TT("dve", R_all[:, hh * 16:(hh + 1) * 16, :].rearrange("p a b -> p (a b)"), pH[hh][:, :],
                   csum[:, hh * 16:(hh + 1) * 16, :].rearrange("p a b -> p (a b)"), ALU.add, r=[b_pH[hh], b_csum], w=[b_R])
            fw.barrier()

        chk(4)
        with ExitStack() as ph:
            nb = SB(ph, "nb", [128, 32], F32); b_nb = Buf()
            cmp3 = SB(ph, "cmp3", [128, 64, 32], F32); b_cmp3 = Buf()
            inc = [SB(ph, "inc%d" % i, [128, 32], F32) for i in range(2)]; b_inc = [Buf(), Buf()]
            pst = SB(ph, "pst", [128, 32], F32); b_pst = Buf()
            bef = SB(ph, "bef", [128, 64], F32); b_bef = Buf()
            trail = SB(ph, "trail", [128, 64], F32); b_trail = Buf()
            ddb = SB(ph, "ddb", [128, NOT, 32], F32); b_ddb = Buf()
            d1b = SB(ph, "d1b", [128, NOT, 32], F32); b_d1b = Buf()
            d2b = SB(ph, "d2b", [128, NOT, 32], F32); b_d2b = Buf()
            mxn = SB(ph, "mxn", [128, 3, NOT], F32); b_mxn = Buf()
            idxf = SB(ph, "idxf", [128, NOT, 2], F32); b_idxf = Buf()
            h2l = [SB(ph, "h2l%d" % i, [128, 1024], BF16) for i in range(4)]; b_h2l = [Buf() for _ in range(4)]
            TT("dve", cmp3[:, 0:32, 0:16], base[:].unsqueeze(2).to_broadcast([128, 32, 16]),
               misc[:, 0:16].unsqueeze(1).to_broadcast([128, 32, 16]), ALU.is_gt, r=[b_base, b_misc], w=[b_cmp3])
            RED(nb[:], cmp3[:, 0:32, 0:16], ALU.add, r=[b_cmp3], w=[b_nb])
            CP("dve", inc[0][:], nb[:], r=[b_nb], w=[b_inc[0]])
            cur = 0
            for sh in (1, 2, 4, 8, 16):
                nx = 1 - cur
                CP("dve", inc[nx][:, 0:sh], inc[cur][:, 0:sh], r=[b_inc[cur]], w=[b_inc[nx]])
                TT("dve", inc[nx][:, sh:32], inc[cur][:, sh:32], inc[cur][:, 0:32 - sh], ALU.add, r=[b_inc[cur]], w=[b_inc[nx]])
                cur = nx
            incl = inc[cur]; b_incl = b_inc[cur]
            TT("dve", pst[:], incl[:], nb[:], ALU.subtract, r=[b_incl, b_nb], w=[b_pst])
            TS("dve", pst[:], pst[:], 256.0, None, ALU.mult, r=[b_pst], w=[b_pst])
            TT("dve", cmp3[:], incl[:].unsqueeze(1).to_broadcast([128, 64, 32]),
               misc[:, 16:80].unsqueeze(2).to_broadcast([128, 64, 32]), ALU.is_le, r=[b_incl, b_misc], w=[b_cmp3])
            RED(bef[:], cmp3[:], ALU.add, r=[b_cmp3], w=[b_bef])
            TS("dve", bef[:], bef[:], 31.0, 128.0, ALU.min, ALU.mult, r=[b_bef], w=[b_bef])
            TS("dve", bef[:], bef[:], misc[:, 80:81], None, ALU.add, r=[b_bef, b_misc], w=[b_bef])
            TS("dve", trail[:], misc[:, 16:80], incl[:, 31:32], 4096.0, ALU.is_ge, ALU.mult, r=[b_misc, b_incl], w=[b_trail])
            TT("dve", bef[:], bef[:], trail[:], ALU.add, r=[b_bef, b_trail], w=[b_bef])
            CP("dve", widx[:], bef[:], r=[b_bef], w=[b_widx])
            R3 = R_all[:]; A3 = A_all[:]; W3 = W_all[:]
            flat = lambda t: t[:].rearrange("p a b -> p (a b)")
            TT("dve", ddb[:], R3, pst[:].unsqueeze(1).to_broadcast([128, NOT, 32]), ALU.add, r=[b_R, b_pst], w=[b_ddb])
            STT(flat(d1b), flat(ddb), 1.0, flat(A_all), ALU.add, ALU.mult, r=[b_ddb, b_A], w=[b_d1b])
            STT(flat(d2b), flat(ddb), -1048576.0, flat(A_all), ALU.add, ALU.mult, r=[b_ddb, b_A], w=[b_d2b])
            RED(mxn[:, 0, :], d1b[:], ALU.max, r=[b_d1b], w=[b_mxn])
            RED(mxn[:, 1, :], d2b[:], ALU.min, r=[b_d2b], w=[b_mxn])
            TS("dve", idxf[:, :, 0], mxn[:, 1, :], 1048576.0, None, ALU.add, r=[b_mxn], w=[b_idxf])
            TS("dve", idxf[:, :, 1], mxn[:, 0, :], -1.0, None, ALU.add, r=[b_mxn], w=[b_idxf])
            CP("dve", flat(idx_all), flat(idxf), r=[b_idxf], w=[b_idx])
            TT("dve", d2b[:], d1b[:], mxn[:, 0, :].unsqueeze(2).to_broadcast([128, NOT, 32]), ALU.is_equal, r=[b_d1b, b_mxn], w=[b_d2b])
            TT("dve", d2b[:], d2b[:], W3, ALU.mult, r=[b_d2b, b_W], w=[b_d2b])
            RED(wsel[:, :, 1], d2b[:], ALU.add, r=[b_d2b], w=[b_wsel])
            RED(mxn[:, 2, :], W3, ALU.add, r=[b_W], w=[b_mxn])
            TT("dve", wsel[:, :, 0], mxn[:, 2, :], wsel[:, :, 1], ALU.subtract, r=[b_mxn, b_wsel], w=[b_wsel])
            NHB = 4
            for ot in range(min(NHB - 1, NOT)):
                fw.dma("sp", h2l[ot % NHB][:], H2_d[ot * 128:(ot + 1) * 128, :], writes=[b_h2l[ot % NHB]])
            for ot in range(NOT):
                j = ot % NHB
                nx = ot + NHB - 1
                if nx < NOT:
                    fw.dma("sp", h2l[nx % NHB][:], H2_d[nx * 128:(nx + 1) * 128, :], writes=[b_h2l[nx % NHB]])
                for k_ in range(2):
                    fw.dma("pool", XS_d[:, :], h2l[j][:], reads=[b_h2l[j], b_idx],
                           indirect=dict(out_offset=bass.IndirectOffsetOnAxis(ap=idx_all[:, ot, k_:k_ + 1], axis=0), in_offset=None))
            if debug:
                rt = SB(ph, "rt", [128, 8], F32); b_rtb = Buf()
                for ot in range(NOT):
                    CP("dve", rt[:, 0:2], idx_all[:, ot, :], r=[b_idx], w=[b_rtb])
                    CP("dve", rt[:, 2:4], wsel[:, ot, :], r=[b_wsel], w=[b_rtb])
                    CP("dve", rt[:, 4:8], widx[:, ot * 2:ot * 2 + 4] if ot < 30 else widx[:, 60:64], r=[b_widx], w=[b_rtb])
                    fw.dma("sp", RT_d[ot * 128:(ot + 1) * 128, :], rt[:], reads=[b_rtb])
            fw.barrier()

        chk(5)
        with ExitStack() as ph:
            wg = [SB(ph, "wg%d" % i, [128, 8, 512], BF16) for i in range(2)]
            wu = [SB(ph, "wu%d" % i, [128, 8, 512], BF16) for i in range(2)]
            wd = [SB(ph, "wd%d" % i, [128, 4, 1024], BF16) for i in range(2)]
            b_wg = [Buf(), Buf()]; b_wu = [Buf(), Buf()]; b_wd = [Buf(), Buf()]
            xs = [SB(ph, "xs%d" % i, [128, 2, 1024], BF16) for i in range(2)]; b_xs = [Buf(), Buf()]
            xT = [SB(ph, "xT%d" % i, [128, 8, 256], BF16) for i in range(2)]; b_xT = [Buf(), Buf()]
            sg = [SB(ph, "sg%d" % i, [128, 256], F32) for i in range(4)]; b_sg = [Buf() for _ in range(4)]
            hidT = [SB(ph, "hidT%d" % i, [128, 4, 256], BF16) for i in range(2)]; b_hidT = [Buf(), Buf()]
            ysb = [SB(ph, "ysb%d" % i, [128, 1024], F32) for i in range(2)]; b_ysb = [Buf(), Buf()]
            pX = [PS(ph, "pX%d" % i, [128, 1024], BF16) for i in range(2)]; b_pX = [PBuf(), PBuf()]
            pG = [PS(ph, "pG%d" % i, [128, 512], F32) for i in range(4)]; b_pG = [PBuf() for _ in range(4)]
            pY = [PS(ph, "pY%d" % i, [128, 512], F32) for i in range(2)]; b_pY = [PBuf(), PBuf()]
            wgv, wuv, wdv = WB_d[0][0:4096, :], WB_d[1][0:4096, :], WB_d[2][0:4096, :]

            bc_reg = nc.gpsimd.to_reg(4095)

            def load_slot(s, part=0):
                j = s % 2
                off = dict(out_offset=None, in_offset=bass.IndirectOffsetOnAxis(ap=widx[:, s:s + 1], axis=0),
                           bounds_check=bc_reg, oob_is_err=False)
                if part in (0, 1):
                    fw.dma("pool", wg[j][:].rearrange("p a b -> p (a b)"), wgv, reads=[b_widx], writes=[b_wg[j]], indirect=off)
                    fw.dma("pool", wu[j][:].rearrange("p a b -> p (a b)"), wuv, reads=[b_widx], writes=[b_wu[j]], indirect=off)
                if part in (0, 2):
                    fw.dma("pool", wd[j][:].rearrange("p a b -> p (a b)"), wdv, reads=[b_widx], writes=[b_wd[j]], indirect=off)
                if part == 2:
                    return
                fw.dma("act", xs[j][:], XS_d[s * 256:(s + 1) * 256, :].rearrange("(r p) f -> p r f", p=128),
                       writes=[b_xs[j]])

            def XT(s):
                j = s % 2
                for r_ in range(2):
                    xv = xs[j][:, r_, :].rearrange("p (a k) -> p k a", k=8)
                    for kc in range(8):
                        TR(pX[r_][:, kc * 128:(kc + 1) * 128], xv[:, kc, :], ident_bf[:], r=[b_xs[j], b_identbf], w=[b_pX[r_]], sig=(kc == 7))
                    CP("act" if r_ == 0 else "dve", xT[j][:, :, r_ * 128:(r_ + 1) * 128],
                       pX[r_][:, :].rearrange("p (a b) -> p a b", a=8), r=[b_pX[r_]], w=[b_xT[j]])

            def GU(s):
                j = s % 2
                wgs = wg[j][:].rearrange("p k (m c) -> p k c m", c=4)
                wus = wu[j][:].rearrange("p k (m c) -> p k c m", c=4)
                for hc in range(4):
                    g_ = hc
                    for kc in range(8):
                        MM(pG[g_][:, 0:256], wgs[:, kc, hc, :], xT[j][:, kc, :], start=(kc == 0), stop=(kc == 7),
                           r=[b_wg[j], b_xT[j]], w=[b_pG[g_]], sig=False)
                    for kc in range(8):
                        MM(pG[g_][:, 256:512], wus[:, kc, hc, :], xT[j][:, kc, :], start=(kc == 0), stop=(kc == 7),
                           r=[b_wu[j], b_xT[j]], w=[b_pG[g_]], sig=(kc == 7))
                    ACT(sg[g_][:], pG[g_][:, 0:256], AF.Silu, r=[b_pG[g_]], w=[b_sg[g_]])
                    TT("dve", hidT[j][:, hc, :], sg[g_][:], pG[g_][:, 256:512], ALU.mult, r=[b_sg[g_], b_pG[g_]], w=[b_hidT[j]])

            yc = [0]

            def DOWN(s):
                j = s % 2
                for r_ in range(2):
                    yb = yc[0] % 2; yc[0] += 1
                    for cg in range(2):
                        for hc in range(4):
                            MM(pY[cg][:, :], hidT[j][:, hc, r_ * 128:(r_ + 1) * 128], wd[j][:, hc, cg * 512:(cg + 1) * 512],
                               start=(hc == 0), stop=(hc == 3), r=[b_hidT[j], b_wd[j]], w=[b_pY[cg]], sig=(hc == 3))
                        CP("act" if cg == 0 else "dve", ysb[yb][:, cg * 512:(cg + 1) * 512], pY[cg][:, :], r=[b_pY[cg]], w=[b_ysb[yb]])
                    row = s * 256 + r_ * 128
                    fw.dma("sp", Y_d[row:row + 128, :], ysb[yb][:], reads=[b_ysb[yb]])

            load_slot(0)
            load_slot(1)
            XT(0)
            for s in range(NSLOT):
                GU(s)
                if s + 1 < NSLOT:
                    XT(s + 1)
                if s + 2 < NSLOT:
                    load_slot(s + 2, 1)
                DOWN(s)
                if s + 2 < NSLOT:
                    load_slot(s + 2, 2)
            fw.barrier()

        chk(6)
        with ExitStack() as ph:
            x1t = [SB(ph, "x1t%d" % i, [128, 1024], F32) for i in range(4)]; b_x1t = [Buf() for _ in range(4)]
            ylo = [SB(ph, "ylo%d" % i, [128, 1024], F32) for i in range(2)]; b_ylo = [Buf(), Buf()]
            yhi = [SB(ph, "yhi%d" % i, [128, 1024], F32) for i in range(2)]; b_yhi = [Buf(), Buf()]
            t1 = [SB(ph, "t1_%d" % i, [128, 1024], F32) for i in range(2)]; b_t1 = [Buf(), Buf()]
            t2 = [SB(ph, "t2_%d" % i, [128, 1024], F32) for i in range(2)]; b_t2 = [Buf(), Buf()]
            t3 = [SB(ph, "t3_%d" % i, [128, 1024], F32) for i in range(2)]; b_t3 = [Buf(), Buf()]
            ssf = [SB(ph, "ssf%d" % i, [128, 8], F32) for i in range(2)]; b_ssf = [Buf(), Buf()]
            xo = [SB(ph, "xo%d" % i, [128, 1024], F32) for i in range(4)]; b_xo = [Buf() for _ in range(4)]
            junk = SB(ph, "junkf", [128, 1024], BF16); b_junk = Buf()
            ss = SB(ph, "ssf", [128, 8], F32); b_ss = Buf()
            ob = [SB(ph, "ob%d" % i, [128, 1024], F32) for i in range(2)]; b_ob = [Buf(), Buf()]
            b_out = Buf()

            def load_f(ot):
                j = ot % 2
                fw.dma("sp", x1t[ot % 4][:], X1_d[ot * 128:(ot + 1) * 128, :], writes=[b_x1t[ot % 4]])
                fw.dma("pool", ylo[j][:], Y_d[:, :], reads=[b_idx], writes=[b_ylo[j]],
                       indirect=dict(out_offset=None, in_offset=bass.IndirectOffsetOnAxis(ap=idx_all[:, ot, 0:1], axis=0)))
                fw.dma("pool", yhi[j][:], Y_d[:, :], reads=[b_idx], writes=[b_yhi[j]],
                       indirect=dict(out_offset=None, in_offset=bass.IndirectOffsetOnAxis(ap=idx_all[:, ot, 1:2], axis=0)))

            def Fa(ot):
                j = ot % 2
                ACT(t1[j][:], ylo[j][:], AF.Copy, r=[b_ylo[j], b_wsel], w=[b_t1[j]], scale=wsel[:, ot, 0:1])
                STT(t2[j][:], yhi[j][:], wsel[:, ot, 1:2], t1[j][:], ALU.mult, ALU.add, r=[b_yhi[j], b_wsel, b_t1[j]], w=[b_t2[j]])

            def Fb(ot):
                j = ot % 2; q = ot % 4
                TT("dve", t3[j][:], t2[j][:], GT2, ALU.mult, r=[b_t2[j], b_mod], w=[b_t3[j]])
                TT("dve", xo[q][:], t3[j][:], x1t[q][:], ALU.add, r=[b_t3[j], b_x1t[q]], w=[b_xo[q]])

            def Fc(ot):
                j = ot % 2; q = ot % 4
                ACT(junk[:], xo[q][:], AF.Square, r=[b_xo[q]], w=[b_junk, b_ssf[j]], accum_out=ssf[j][:, 0:1])
                TS("dve", ssf[j][:, 1:2], ssf[j][:, 0:1], 1.0 / 1024, 1e-6, ALU.mult, ALU.add, r=[b_ssf[j]], w=[b_ssf[j]])
                TT("pool", ssf[j][:, 2:3], ssf[j][:, 1:2], misc[:, 81:82], ALU.pow, r=[b_ssf[j], b_misc], w=[b_ssf[j]])

            def Fd(ot):
                j = ot % 2; q = ot % 4
                STT(ob[j][:], xo[q][:], ssf[j][:, 2:3], gfin[:], ALU.mult, ALU.mult, r=[b_xo[q], b_ssf[j], b_gfin], w=[b_ob[j]])
                fw.dma("sp", out[ot * 128:(ot + 1) * 128, :], ob[j][:], reads=[b_ob[j]], writes=[b_out])

            load_f(0)
            for k in range(NOT + 3):
                if k + 1 < NOT:
                    load_f(k + 1)
                if 0 <= k < NOT: Fa(k)
                if 0 <= k - 1 < NOT: Fb(k - 1)
                if 0 <= k - 2 < NOT: Fc(k - 2)
                if 0 <= k - 3 < NOT: Fd(k - 3)
            fw.barrier()
      except _Stop:
        pass
      stats = {k: (e.n_instr, e.cnt) for k, e in gs_fw[0].engs.items()}
    return nc, stats


def _own_chunks(par):
    own, oth = [], []
    for i in range(NPAIR):
        a, b = 2 * i, 2 * i + 1
        if (i % 2 == 0) == (par == 0):
            own.append(a); oth.append(b)
        else:
            own.append(b); oth.append(a)
    return own, oth


def _rot_table():
    half = 8
    inv = np.power(np.float32(500000.0), -np.arange(half, dtype=np.float32) * np.float32(2.0) / np.float32(16)).astype(np.float32)
    ang = (np.arange(8192, dtype=np.float32)[:, None] * inv[None, :]).astype(np.float32)
    cos = np.cos(ang).astype(np.float32)
    sin = np.sin(ang).astype(np.float32)
    return np.concatenate([np.tile(cos, (1, 8)), np.tile(sin, (1, 8))], axis=1).astype(np.float32)


def make_in_maps(inputs):
    f = lambda a: np.ascontiguousarray(np.asarray(a, dtype=np.float32))
    x = f(inputs["x"]); c = f(inputs["c"])
    rot = _rot_table()
    ident = np.eye(128, dtype=np.float32)
    kk = np.arange(128)
    tri = (kk[:, None] <= kk[None, :]).astype(np.float32)
    tris = (kk[:, None] < kk[None, :]).astype(np.float32)
    onehot = (np.arange(8192)[None, :] // 256 == np.arange(32)[:, None]).astype(np.float32)
    misc = np.zeros((128, 96), np.float32)
    misc[:, 0:16] = 256.0 * np.arange(16)[None, :]
    misc[:, 16:80] = np.arange(64)[None, :]
    misc[:, 80] = np.arange(128)
    misc[:, 81] = -0.5
    w_rt = np.concatenate([f(inputs["w_router_group"])[0], f(inputs["w_router_expert"])[0].reshape(1024, 32)], axis=1)
    b_rt = np.concatenate([f(inputs["b_router_group"])[0], f(inputs["b_router_expert"])[0].reshape(32)])[None, :]
    shared = dict(
        ada_w=f(inputs["ada_w"])[0], ada_b=f(inputs["ada_b"]), norm1_g=f(inputs["norm1_g"]), norm2_g=f(inputs["norm2_g"]),
        final_g=f(inputs["final_norm_g"])[None, :], w_in=f(inputs["w_in"])[0], ln_g=f(inputs["gmlp_ln_g"]),
        ln_b=f(inputs["gmlp_ln_b"]), w_sp=f(inputs["w_spatial"])[0], b_spT=np.ascontiguousarray(f(inputs["b_spatial"])[0].T),
        w_ba=f(inputs["w_branch_a"])[0], w_bb=f(inputs["w_branch_b"])[0], w_out=f(inputs["w_out"])[0],
        w_rt=np.ascontiguousarray(w_rt), b_rt=np.ascontiguousarray(b_rt),
        w_gate=f(inputs["w_gate"])[0], w_up=f(inputs["w_up"])[0], w_down=f(inputs["w_down"])[0],
        c_ident=ident, c_tri=tri, c_tris=tris, c_onehot=onehot, c_misc=misc)
    maps, rowmaps = [], []
    for core in range(8):
        b, par = core // 2, core % 2
        own, oth = _own_chunks(par)
        rows_own = np.concatenate([np.arange(ch * 256, (ch + 1) * 256) for ch in own])
        rows_oth = np.concatenate([np.arange(ch * 256, (ch + 1) * 256) for ch in oth])
        pb_ = np.full((16, 32), -1e30, np.float32)
        for i in range(NPAIR):
            pb_[i, 0:2 * i] = 0.0
            if own[i] > oth[i]:
                pb_[i, 2 * i] = 0.0
        m = dict(shared)
        m.update(x_own=np.ascontiguousarray(x[b][rows_own]), x_oth=np.ascontiguousarray(x[b][rows_oth]),
                 rot_own=np.ascontiguousarray(rot[rows_own]), rot_oth=np.ascontiguousarray(rot[rows_oth]),
                 pastb=pb_.reshape(1, 512), c_pk=np.ascontiguousarray(c[b].reshape(8, 128).T))
        maps.append(m)
        rowmaps.append((b, rows_own))
    return maps, rowmaps


_CACHE = {}


def kernel(**inputs):
    if "nc" not in _CACHE:
        _CACHE["nc"] = build(False)[0]
    nc = _CACHE["nc"]
    maps, rowmaps = make_in_maps(inputs)
    res = run_bass_kernel_spmd(nc, maps, core_ids=list(range(8)))
    outp = np.empty((4, 8192, 1024), np.float32)
    for core in range(8):
        b, rows = rowmaps[core]
        outp[b, rows] = np.asarray(res.results[core]["out"], dtype=np.float32)
    return outp
```
